# Optimizing a Trainium2 kernel written in Bass

```python
import math
import jax, jax.numpy as jnp
from jax import lax
import numpy as np

D_MODEL = 1024
BATCH = 8
SEQ = 4096
DEPTH = 1

D_MIX = D_MODEL
HEAD_DIM = 64
ATTN_HEADS = (D_MIX // 2) // HEAD_DIM
ATTN_DIM = ATTN_HEADS * HEAD_DIM
CONV_DIM = D_MIX - ATTN_DIM
CONV_K = 3
IDX_HEADS = 8
IDX_DIM = 64
IDX_SCALE = (IDX_DIM ** -0.5) * (IDX_HEADS ** -0.5)
TOPK_KEYS_MAX = 256
QUERY_BLOCK = 128
ROPE_THETA = 500000.0
ROPE_DIM = HEAD_DIM // 4
N_EXPERTS = 64
TOP_K_EXPERTS = 8
EXPERT_DIM = 256
SHARED_DIM = 256
ROUTED_SCALE = 2.5
DISPATCH_BLOCK = 128
EPS = 1e-6
IN_SPLITS = (CONV_DIM, CONV_DIM, CONV_DIM, ATTN_DIM, ATTN_DIM, ATTN_DIM,
             IDX_HEADS * IDX_DIM, IDX_DIM, IDX_HEADS)
IN_COLS = sum(IN_SPLITS)

kernel_name = "hymba_conv_dsa_moe_adaln_block"


def rms_norm(x, g):
    xf = x.astype(jnp.float32)
    y = xf * lax.rsqrt(jnp.mean(xf * xf, axis=-1, keepdims=True) + EPS)
    return y.astype(x.dtype) * g


def apply_partial_rope(x, positions):
    half = ROPE_DIM // 2
    inv_freq = ROPE_THETA ** (-jnp.arange(half, dtype=jnp.float32) / half)
    ang = positions.astype(jnp.float32)[..., None] * inv_freq
    ang = ang.reshape(ang.shape[:2] + (1,) * (x.ndim - 3) + (half,))
    cos, sin = jnp.cos(ang), jnp.sin(ang)
    x1 = x[..., :half].astype(jnp.float32)
    x2 = x[..., half:ROPE_DIM].astype(jnp.float32)
    rot = jnp.concatenate([x1 * cos - x2 * sin, x2 * cos + x1 * sin], axis=-1)
    return jnp.concatenate([rot.astype(x.dtype), x[..., ROPE_DIM:]], axis=-1)


def short_conv(u, w):
    up = jnp.pad(u, ((0, 0), (CONV_K - 1, 0), (0, 0)))
    return up[:, :-2] * w[0] + up[:, 1:-1] * w[1] + up[:, 2:] * w[2]


def dsa_attention(q, k, v, qi, ki, wi):
    B, S, H, Dh = q.shape
    n_sel = min(TOPK_KEYS_MAX, S // 4)
    n_blocks = S // QUERY_BLOCK
    key_pos = jnp.arange(S)

    def block(j):
        q0 = j * QUERY_BLOCK
        qb = lax.dynamic_slice_in_dim(q, q0, QUERY_BLOCK, axis=1)
        qib = lax.dynamic_slice_in_dim(qi, q0, QUERY_BLOCK, axis=1)
        wib = lax.dynamic_slice_in_dim(wi, q0, QUERY_BLOCK, axis=1)
        t_pos = q0 + jnp.arange(QUERY_BLOCK)
        rel = jax.nn.relu(jnp.einsum('bqhd,bsd->bqhs', qib, ki).astype(jnp.float32))
        score = jnp.einsum('bqhs,bqh->bqs', rel, wib.astype(jnp.float32)) * IDX_SCALE
        causal = key_pos[None, :] <= t_pos[:, None]
        score = jnp.where(causal[None], score, -jnp.inf)
        _, idx = lax.top_k(score, n_sel)
        valid = idx <= t_pos[None, :, None]
        flat = idx.reshape(B, QUERY_BLOCK * n_sel)
        ks = jax.vmap(lambda kb, ib: kb[ib])(k, flat).reshape(B, QUERY_BLOCK, n_sel, H, Dh)
        vs = jax.vmap(lambda vb, ib: vb[ib])(v, flat).reshape(B, QUERY_BLOCK, n_sel, H, Dh)
        s = jnp.einsum('bqhd,bqkhd->bhqk', qb, ks).astype(jnp.float32) * (Dh ** -0.5)
        s = jnp.where(valid[:, None], s, -jnp.inf)
        p = jax.nn.softmax(s, axis=-1).astype(vs.dtype)
        o = jnp.einsum('bhqk,bqkhd->bqhd', p, vs)
        return o.reshape(B, QUERY_BLOCK, H * Dh)

    out = lax.map(block, jnp.arange(n_blocks))
    return out.transpose(1, 0, 2, 3).reshape(B, S, H * Dh)


def swiglu(x, w1, w3, w2):
    return (jax.nn.silu(x @ w1) * (x @ w3)) @ w2


def moe(h, w_router, router_bias, w1, w3, w2, ws1, ws3, ws2):
    B, S, D = h.shape
    t = h.reshape(-1, D)
    n_tok = t.shape[0]
    scores = jax.nn.sigmoid((t @ w_router).astype(jnp.float32))
    _, eidx = lax.top_k(scores + router_bias.astype(jnp.float32), TOP_K_EXPERTS)
    g = jnp.take_along_axis(scores, eidx, axis=1)
    g = g / jnp.sum(g, axis=-1, keepdims=True) * ROUTED_SCALE
    n_pair = n_tok * TOP_K_EXPERTS
    flat_e = eidx.reshape(-1)
    flat_tok = jnp.arange(n_pair, dtype=jnp.int32) // TOP_K_EXPERTS
    flat_g = g.reshape(-1).astype(t.dtype)
    order = jnp.argsort(flat_e)
    se, stok, sg = flat_e[order], flat_tok[order], flat_g[order]
    counts = jnp.bincount(flat_e, length=N_EXPERTS)
    padded = (counts + DISPATCH_BLOCK - 1) // DISPATCH_BLOCK * DISPATCH_BLOCK
    start = jnp.cumsum(counts) - counts
    pend = jnp.cumsum(padded)
    pstart = pend - padded
    dest = pstart[se] + jnp.arange(n_pair) - start[se]
    rows = n_pair + N_EXPERTS * DISPATCH_BLOCK
    n_blk = rows // DISPATCH_BLOCK
    row_tok = jnp.zeros((rows,), jnp.int32).at[dest].set(stok)
    row_g = jnp.zeros((rows,), t.dtype).at[dest].set(sg)
    block_e = jnp.clip(jnp.searchsorted(pend, jnp.arange(n_blk) * DISPATCH_BLOCK, side='right'),
                       0, N_EXPERTS - 1)

    def expert_block(args):
        e, toks, gs = args
        xb = t[toks]
        return swiglu(xb, w1[e], w3[e], w2[e]) * gs[:, None]

    out = lax.map(expert_block, (block_e, row_tok.reshape(n_blk, DISPATCH_BLOCK),
                                 row_g.reshape(n_blk, DISPATCH_BLOCK)))
    routed = jax.ops.segment_sum(out.reshape(rows, D), row_tok, num_segments=n_tok)
    shared = swiglu(t, ws1, ws3, ws2)
    return (routed + shared).reshape(B, S, D)


def hybrid_layer(x, c, positions, norm1_g, norm2_g, w_ada, b_ada, w_in, conv_w,
                 q_norm_g, k_norm_g, kidx_norm_g, w_out, w_router, router_bias,
                 w1, w3, w2, ws1, ws3, ws2):
    B, S, D = x.shape
    ada = jax.nn.silu(c) @ w_ada + b_ada
    shift1, scale1, gate1, shift2, scale2, gate2 = [a[:, None, :] for a in jnp.split(ada, 6, axis=-1)]

    h = rms_norm(x, norm1_g) * (1 + scale1) + shift1
    proj = h @ w_in
    cuts = list(np.cumsum(IN_SPLITS)[:-1])
    xc, bg, cg, q, k, v, qi, ki, wi = jnp.split(proj, cuts, axis=-1)
    conv_out = bg * short_conv(cg * xc, conv_w)
    q = apply_partial_rope(rms_norm(q.reshape(B, S, ATTN_HEADS, HEAD_DIM), q_norm_g), positions)
    k = apply_partial_rope(rms_norm(k.reshape(B, S, ATTN_HEADS, HEAD_DIM), k_norm_g), positions)
    v = v.reshape(B, S, ATTN_HEADS, HEAD_DIM)
    qi = apply_partial_rope(qi.reshape(B, S, IDX_HEADS, IDX_DIM), positions)
    ki = apply_partial_rope(rms_norm(ki, kidx_norm_g), positions)
    attn_out = dsa_attention(q, k, v, qi, ki, wi)
    mix = jnp.concatenate([conv_out, attn_out], axis=-1) @ w_out
    x = x + gate1 * mix

    h2 = rms_norm(x, norm2_g) * (1 + scale2) + shift2
    x = x + gate2 * moe(h2, w_router, router_bias, w1, w3, w2, ws1, ws3, ws2)
    return x


def setup_inputs(seed: int = 0) -> dict:
    key = jax.random.key(seed)
    ks = jax.random.split(key, 24)
    f32 = jnp.float32
    L, D, E, F, Fs = DEPTH, D_MODEL, N_EXPERTS, EXPERT_DIM, SHARED_DIM

    def nrm(k, shape, scale):
        return jax.random.normal(k, shape, f32) * scale

    positions = (jax.random.randint(ks[2], (BATCH, 1), 0, 1024, dtype=jnp.int32)
                 + jnp.arange(SEQ, dtype=jnp.int32)[None, :])
    return {
        "x": nrm(ks[0], (BATCH, SEQ, D), 1.0),
        "c": nrm(ks[1], (BATCH, D), 1.0),
        "positions": positions,
        "norm1_g": 1.0 + nrm(ks[3], (L, D), 0.02),
        "norm2_g": 1.0 + nrm(ks[4], (L, D), 0.02),
        "w_ada": nrm(ks[5], (L, D, 6 * D), 0.5 * D ** -0.5),
        "b_ada": nrm(ks[6], (L, 6 * D), 0.02),
        "w_in": nrm(ks[7], (L, D, IN_COLS), D ** -0.5),
        "conv_w": nrm(ks[8], (L, CONV_K, CONV_DIM), CONV_K ** -0.5),
        "q_norm_g": 1.0 + nrm(ks[9], (L, HEAD_DIM), 0.02),
        "k_norm_g": 1.0 + nrm(ks[10], (L, HEAD_DIM), 0.02),
        "kidx_norm_g": 1.0 + nrm(ks[11], (L, IDX_DIM), 0.02),
        "w_out": nrm(ks[12], (L, D_MIX, D), D_MIX ** -0.5),
        "w_router": nrm(ks[13], (L, D, E), D ** -0.5),
        "router_bias": nrm(ks[14], (L, E), 0.01),
        "w1": nrm(ks[15], (L, E, D, F), D ** -0.5),
        "w3": nrm(ks[16], (L, E, D, F), D ** -0.5),
        "w2": nrm(ks[17], (L, E, F, D), F ** -0.5),
        "ws1": nrm(ks[18], (L, D, Fs), D ** -0.5),
        "ws3": nrm(ks[19], (L, D, Fs), D ** -0.5),
        "ws2": nrm(ks[20], (L, Fs, D), Fs ** -0.5),
    }


def reference(x, c, positions, norm1_g, norm2_g, w_ada, b_ada, w_in, conv_w,
              q_norm_g, k_norm_g, kidx_norm_g, w_out, w_router, router_bias,
              w1, w3, w2, ws1, ws3, ws2):
    for l in range(DEPTH):
        x = hybrid_layer(x, c, positions, norm1_g[l], norm2_g[l], w_ada[l], b_ada[l],
                         w_in[l], conv_w[l], q_norm_g[l], k_norm_g[l], kidx_norm_g[l],
                         w_out[l], w_router[l], router_bias[l], w1[l], w3[l], w2[l],
                         ws1[l], ws3[l], ws2[l])
    return x
```

```python
import math
from contextlib import ExitStack

import numpy as np
import concourse.bass as bass
import concourse.mybir as mybir
from concourse.bass_utils import run_bass_kernel_spmd

F32 = mybir.dt.float32
BF16 = mybir.dt.bfloat16
I32 = mybir.dt.int32
AF = mybir.ActivationFunctionType
ALU = mybir.AluOpType
AX = mybir.AxisListType

S = 4096
D = 1024
NT = S // 128
NE = 64
BS = 256
NB = (S * 8 + NE * BS) // BS
NSLOT = NB * BS
NIT = 14
NSEL = 256
IN_COLS = 3656
TWO_PI = 2.0 * math.pi


class Eng:
    def __init__(self, name, h, sem):
        self.name, self.h, self.sem, self.n, self.seen = name, h, sem, 0, {}


class Res:
    __slots__ = ("w", "r")

    def __init__(self):
        self.w = None
        self.r = {}


class DS:
    def __init__(self, sem):
        self.sem, self.val = sem, 0


class Buf:
    def __init__(self, t):
        self.t = t
        self.r = Res()


class KB:
    def __init__(self, nc, es):
        self.nc, self.es = nc, es
        mk = lambda n, h: Eng(n, h, es.enter_context(nc.semaphore("sem_" + n)))
        self.pe = mk("pe", nc.tensor)
        self.act = mk("act", nc.scalar)
        self.dve = mk("dve", nc.vector)
        self.pool = mk("pool", nc.gpsimd)
        self.sp = mk("sp", nc.sync)
        self.engs = [self.pe, self.act, self.dve, self.pool, self.sp]
        self.dss = []
        self.nbuf = 0

    def ds(self):
        d = DS(self.es.enter_context(self.nc.semaphore("ds%d" % len(self.dss))))
        self.dss.append(d)
        return d

    def sb(self, shape, dt, es=None):
        self.nbuf += 1
        return Buf((es or self.es).enter_context(self.nc.sbuf_tensor("sb%d" % self.nbuf, shape, dt)))

    def _sync(self, eng, reads, writes):
        tags = []
        for r in reads:
            if r.w is not None:
                tags.append(r.w)
        for w in writes:
            if w.w is not None:
                tags.append(w.w)
            tags.extend(w.r.values())
        for (kind, obj, val) in tags:
            if kind == "e" and obj is eng and eng is self.pe:
                continue
            key = id(obj)
            if eng.seen.get(key, 0) >= val:
                continue
            eng.h.wait_ge(obj.sem, val)
            eng.seen[key] = val

    def I(self, eng, reads, writes, fn):
        reads = [b.r if isinstance(b, Buf) else b for b in reads]
        writes = [b.r if isinstance(b, Buf) else b for b in writes]
        self._sync(eng, reads, writes)
        ins = fn()
        eng.n += 1
        ins.then_inc(eng.sem, 1)
        tag = ("e", eng, eng.n)
        for r in reads:
            r.r[id(eng)] = tag
        for w in writes:
            w.w = tag
            w.r = {}
        return ins

    def dma(self, q, ds, reads, writes, fn):
        reads = [b.r if isinstance(b, Buf) else b for b in reads]
        writes = [b.r if isinstance(b, Buf) else b for b in writes]
        self._sync(q, reads, writes)
        ins = fn()
        ds.val += 16
        ins.then_inc(ds.sem, 16)
        tag = ("d", ds, ds.val)
        for r in reads:
            r.r[id(ds)] = tag
        for w in writes:
            w.w = tag
            w.r = {}
        return ins

    def barrier(self):
        for e in self.engs:
            for f in self.engs:
                if f.n == 0:
                    continue
                if e.seen.get(id(f), 0) < f.n:
                    e.h.wait_ge(f.sem, f.n)
                    e.seen[id(f)] = f.n
            for d in self.dss:
                if d.val and e.seen.get(id(d), 0) < d.val:
                    e.h.wait_ge(d.sem, d.val)
                    e.seen[id(d)] = d.val


def build(stop_after="G", taps=()):
    nc = bass.Bass("TRN2", target_bir_lowering=False)
    dti = lambda n, sh, dt: nc.dram_tensor(n, sh, dt, kind="ExternalInput").ap()
    dts = lambda n, sh, dt: nc.dram_tensor(n, sh, dt, kind="Internal").ap()
    x_d = dti("x", [S, D], F32)
    cT_d = dti("cT", [128, 8], F32)
    pos_d = dti("posT", [128, NT], I32)
    g1_d = dti("norm1_g", [1, D], F32)
    g2_d = dti("norm2_g", [1, D], F32)
    wada_d = dti("w_ada", [D, 6 * D], F32)
    bada_d = dti("b_ada", [1, 6 * D], F32)
    win_d = dti("w_in", [D, IN_COLS], F32)
    convw_d = dti("convw", [128, 12], F32)
    qg_d = dti("qg8", [1, 512], F32)
    kg_d = dti("kg8", [1, 512], F32)
    kig_d = dti("kig", [1, 64], F32)
    wout_d = dti("w_out", [D, D], F32)
    wr_d = dti("w_router", [D, NE], F32)
    rb_d = dti("router_bias", [1, NE], F32)
    w1_d = dti("w1", [NE, D, 256], F32)
    w3_d = dti("w3", [NE, D, 256], F32)
    w2_d = dti("w2", [NE, 256, D], F32)
    ws1_d = dti("ws1", [D, 256], F32)
    ws3_d = dti("ws3", [D, 256], F32)
    ws2_d = dti("ws2", [256, D], F32)
    out_d = nc.dram_tensor("out", [S, D], F32, kind="ExternalOutput").ap()
    tap_d = {n: nc.dram_tensor("tap_" + n, list(sh), dt, kind="ExternalOutput").ap() for (n, sh, dt) in taps}

    QT = dts("QT", [NT, 128, 512], BF16)
    QIT = dts("QIT", [NT, 128, 512], BF16)
    CONVT = dts("CONVT", [NT, 128, 512], BF16)
    ATTNT = dts("ATTNT", [NT, 128, 512], BF16)
    H2 = dts("H2", [S, D], BF16)
    BASE = dts("BASE", [S, D], F32)
    LIST = dts("LIST", [NSLOT, 1], I32)
    YS = dts("YS", [NSLOT, D], F32)
    WB13 = dts("WB13", [NE * 128, 4096], BF16)
    WB2 = dts("WB2", [NE * 128, 2048], BF16)
    ADA = dts("ADA", [128, 6 * D], F32)

    order = "ACDEFG"
    upto = lambda ph: order.index(ph) <= order.index(stop_after)

    with ExitStack() as es:
        kb = KB(nc, es)
        pe, act, dve, pool, sp = kb.pe, kb.act, kb.dve, kb.pool, kb.sp
        V, A, G, T = nc.vector, nc.scalar, nc.gpsimd, nc.tensor

        def tap(name, src_ap, reads, dst=None):
            if name not in tap_d:
                return
            d = kb.ds()
            kb.dma(sp, d, reads, [], lambda: nc.sync.dma_start(out=dst if dst is not None else tap_d[name], in_=src_ap))

        bk = [Buf(es.enter_context(nc.psum_tensor("bk%d" % i, [128, 512], F32))) for i in range(7)]
        PT = Buf(es.enter_context(nc.psum_tensor("PT", [128, 1024], BF16)))

        ident = kb.sb([128, 128], BF16)
        ident30 = kb.sb([128, 128], BF16)
        ones_bf = kb.sb([128, 128], BF16)
        ustrict = kb.sb([128, 128], BF16)
        ones_f = kb.sb([128, 128], F32)
        cbias = kb.sb([128, 128], BF16)
        eps = kb.sb([128, 1], F32)
        tok_all = kb.sb([128, NT], I32)
        piota = kb.sb([128, 1], F32)
        qg_bc = kb.sb([128, 512], F32)
        kg_bc = kb.sb([128, 512], F32)
        kig_bc = kb.sb([128, 64], F32)
        rb_bc = kb.sb([128, 64], F32)
        convw = kb.sb([128, 12], F32)
        wi_all = kb.sb([128, NT, 8], F32)
        g8_all = kb.sb([128, NT, 8], F32)
        s8_all = kb.sb([128, NT, 8], I32)
        cum_bc = kb.sb([128, NE], F32)
        widx = kb.sb([128, NB], I32)

        cds = kb.ds()

        def cload(q, buf, src, qh):
            kb.dma(q, kb.ds(), [], [buf], lambda: qh.dma_start(out=buf.t[:], in_=src))

        kb.I(pool, [], [ident], lambda: G.memset(ident.t[:], 1.0))
        kb.I(pool, [ident], [ident], lambda: G.affine_select(out=ident.t[:], in_=ident.t[:], pattern=[[1, 128]], compare_op=ALU.is_equal, fill=0.0, base=0, channel_multiplier=-1))
        kb.I(pool, [], [ident30], lambda: G.memset(ident30.t[:], 30000.0))
        kb.I(pool, [ident30], [ident30], lambda: G.affine_select(out=ident30.t[:], in_=ident30.t[:], pattern=[[1, 128]], compare_op=ALU.is_equal, fill=0.0, base=0, channel_multiplier=-1))
        kb.I(pool, [], [ones_bf], lambda: G.memset(ones_bf.t[:], 1.0))
        kb.I(pool, [], [ustrict], lambda: G.memset(ustrict.t[:], 1.0))
        kb.I(pool, [ustrict], [ustrict], lambda: G.affine_select(out=ustrict.t[:], in_=ustrict.t[:], pattern=[[1, 128]], compare_op=ALU.is_gt, fill=0.0, base=0, channel_multiplier=-1))
        kb.I(pool, [], [ones_f], lambda: G.memset(ones_f.t[:], 1.0))
        kb.I(pool, [], [cbias], lambda: G.memset(cbias.t[:], 0.0))
        kb.I(pool, [cbias], [cbias], lambda: G.affine_select(out=cbias.t[:], in_=cbias.t[:], pattern=[[-1, 128]], compare_op=ALU.is_ge, fill=-1e30, base=0, channel_multiplier=1))
        kb.I(pool, [], [eps], lambda: G.memset(eps.t[:], 1e-6))
        kb.I(pool, [], [tok_all], lambda: G.iota(tok_all.t[:], pattern=[[128, NT]], base=0, channel_multiplier=1))
        kb.I(pool, [], [piota], lambda: G.iota(piota.t[:], pattern=[[0, 1]], base=0, channel_multiplier=1, allow_small_or_imprecise_dtypes=True))
        kb.I(pool, [], [cum_bc], lambda: G.memset(cum_bc.t[:], 0.0))

        cload(sp, qg_bc, qg_d.partition_broadcast(128), nc.sync)
        cload(sp, kg_bc, kg_d.partition_broadcast(128), nc.sync)
        cload(sp, kig_bc, kig_d.partition_broadcast(128), nc.sync)
        cload(sp, rb_bc, rb_d.partition_broadcast(128), nc.sync)
        cload(sp, convw, convw_d[:, :], nc.sync)

        with ExitStack() as pa:
            ada = kb.sb([128, 6 * D], F32, pa)
            cT = kb.sb([128, 8], F32, pa)
            sg = kb.sb([128, 8], F32, pa)
            sc = kb.sb([128, 8], F32, pa)
            sc_bc = kb.sb([128, 8, 128], F32, pa)
            bada_bc = kb.sb([128, 6 * D], F32, pa)
            g_bc = kb.sb([128, 2 * D], F32, pa)
            wa = [kb.sb([128, 3 * D], F32, pa) for _ in range(2)]
            wads = [kb.ds(), kb.ds()]
            cload(sp, cT, cT_d[:, :], nc.sync)
            cload(sp, bada_bc, bada_d.partition_broadcast(128), nc.sync)
            gds_a = kb.ds()
            kb.dma(sp, gds_a, [], [g_bc], lambda: nc.sync.dma_start(out=g_bc.t[:, 0:D], in_=g1_d.partition_broadcast(128)))
            kb.dma(sp, gds_a, [g_bc], [g_bc], lambda: nc.sync.dma_start(out=g_bc.t[:, D:2 * D], in_=g2_d.partition_broadcast(128)))
            kb.I(act, [cT], [sg], lambda: A.activation(out=sg.t[:], in_=cT.t[:], func=AF.Sigmoid))
            kb.I(dve, [cT, sg], [sc], lambda: V.tensor_tensor(out=sc.t[:], in0=cT.t[:], in1=sg.t[:], op=ALU.mult))
            for kc in range(8):
                kb.I(dve, [sc, ones_f], [sc_bc], lambda: V.tensor_scalar(out=sc_bc.t[:, kc, :], in0=ones_f.t[:], scalar1=sc.t[:, kc:kc + 1], scalar2=None, op0=ALU.mult))
            it = 0
            for half in range(2):
                for kc in range(8):
                    wb_ = wa[it % 2]
                    kb.dma(sp, wads[it % 2], [], [wb_], lambda: nc.sync.dma_start(out=wb_.t[:], in_=wada_d[kc * 128:(kc + 1) * 128, half * 3 * D:(half + 1) * 3 * D]))
                    for g in range(6):
                        kb.I(pe, [sc_bc, wb_], [bk[g]], lambda: T.matmul(bk[g].t[:], lhsT=sc_bc.t[:, kc, :], rhs=wb_.t[:, g * 512:(g + 1) * 512], start=(kc == 0), stop=(kc == 7)))
                    it += 1
                for g in range(6):
                    c0 = half * 3 * D + g * 512
                    kb.I(dve, [bk[g], bada_bc], [ada], lambda: V.tensor_tensor(out=ada.t[:, c0:c0 + 512], in0=bk[g].t[:], in1=bada_bc.t[:, c0:c0 + 512], op=ALU.add))
            kb.I(dve, [ada, g_bc], [ada], lambda: V.scalar_tensor_tensor(out=ada.t[:, D:2 * D], in0=ada.t[:, D:2 * D], scalar=1.0, in1=g_bc.t[:, 0:D], op0=ALU.add, op1=ALU.mult))
            kb.I(dve, [ada, g_bc], [ada], lambda: V.scalar_tensor_tensor(out=ada.t[:, 4 * D:5 * D], in0=ada.t[:, 4 * D:5 * D], scalar=1.0, in1=g_bc.t[:, D:2 * D], op0=ALU.add, op1=ALU.mult))
            tap("ada", ada.t[0:1, :], [ada])
            kb.dma(sp, cds, [ada], [], lambda: nc.sync.dma_start(out=ADA[:, :], in_=ada.t[:]))
            kb.barrier()

        kside = ExitStack()
        kT = kb.sb([128, 4, S], BF16, kside)
        kiT2 = kb.sb([128, S], BF16, kside)
        Vp = kb.sb([128, NT, 4, 160], BF16, kside)
        kT_r = [Res() for _ in range(NT)]
        kiT_r = [Res() for _ in range(NT)]
        Vp_r = [Res() for _ in range(NT)]

        def rms_small(src_ap, n, scale, rs_out):
            pass

        if upto("C"):
          with ExitStack() as pc:
            Wbf = kb.sb([128, 8, IN_COLS], BF16, pc)
            ada = kb.sb([128, 2 * D], F32, pc)
            SHIFT1, A1 = slice(0, D), slice(D, 2 * D)
            cload(sp, ada, ADA[:, 0:2 * D], nc.sync)
            sincos = kb.sb([128, NT, 16], F32, pc)
            wds = kb.ds()
            for kc in range(8):
                kb.dma(pool, wds, [], [Wbf], lambda: G.dma_start(out=Wbf.t[:, kc, :], in_=win_d[kc * 128:(kc + 1) * 128, :], max_dma_last_dim=4096))
            prs = ExitStack()
            posi = kb.sb([128, NT], I32, prs)
            posf = kb.sb([128, NT], F32, prs)
            ang = kb.sb([128, NT, 16], F32, prs)
            angn = kb.sb([128, NT, 16], F32, prs)
            angi = kb.sb([128, NT, 16], I32, prs)
            angm = kb.sb([128, NT, 16], F32, prs)
            cload(sp, posi, pos_d[:, :], nc.sync)
            kb.I(dve, [posi], [posf], lambda: V.tensor_copy(out=posf.t[:], in_=posi.t[:]))
            for j in range(8):
                inv = float(np.float32(500000.0) ** np.float32(-j / 8.0))
                inv = float(np.float32(inv))
                kb.I(dve, [posf], [ang], lambda: V.tensor_scalar(out=ang.t[:, :, j], in0=posf.t[:], scalar1=inv, scalar2=None, op0=ALU.mult))
            kb.I(dve, [ang], [ang], lambda: V.tensor_scalar(out=ang.t[:, :, 8:16], in0=ang.t[:, :, 0:8], scalar1=math.pi / 2, scalar2=None, op0=ALU.add))
            kb.I(dve, [ang], [angn], lambda: V.tensor_scalar(out=angn.t[:], in0=ang.t[:], scalar1=1.0 / TWO_PI, scalar2=None, op0=ALU.mult))
            kb.I(dve, [angn], [angi], lambda: V.tensor_copy(out=angi.t[:], in_=angn.t[:]))
            kb.I(dve, [angi], [angn], lambda: V.tensor_copy(out=angn.t[:], in_=angi.t[:]))
            kb.I(dve, [angn, ang], [ang], lambda: V.scalar_tensor_tensor(out=ang.t[:], in0=angn.t[:], scalar=-TWO_PI, in1=ang.t[:], op0=ALU.mult, op1=ALU.add))
            kb.I(dve, [ang], [angm], lambda: V.tensor_scalar(out=angm.t[:], in0=ang.t[:], scalar1=-math.pi, scalar2=TWO_PI, op0=ALU.is_lt, op1=ALU.mult))
            kb.I(dve, [ang, angm], [ang], lambda: V.tensor_tensor(out=ang.t[:], in0=ang.t[:], in1=angm.t[:], op=ALU.add))
            kb.I(dve, [ang], [angm], lambda: V.tensor_scalar(out=angm.t[:], in0=ang.t[:], scalar1=math.pi, scalar2=-TWO_PI, op0=ALU.is_gt, op1=ALU.mult))
            kb.I(dve, [ang, angm], [ang], lambda: V.tensor_tensor(out=ang.t[:], in0=ang.t[:], in1=angm.t[:], op=ALU.add))
            kb.I(dve, [ang], [ang], lambda: V.tensor_scalar(out=ang.t[:], in0=ang.t[:], scalar1=3.1415925, scalar2=-3.1415925, op0=ALU.min, op1=ALU.max))
            kb.I(act, [ang], [sincos], lambda: A.activation(out=sincos.t[:], in_=ang.t[:], func=AF.Sin))
            tap("sincos", sincos.t[:], [sincos])
            kb.barrier()
            prs.close()
            kb.I(pool, [], Vp_r, lambda: G.memset(Vp.t[:], 0.0))
            kb.I(pool, [], Vp_r, lambda: G.memset(Vp.t[:, :, :, 64:65], 1.0))

            xbuf = [kb.sb([128, D], F32, pc) for _ in range(2)]
            xds = [kb.ds(), kb.ds()]
            ss = kb.sb([128, 1], F32, pc)
            sd = kb.sb([128, 1], F32, pc)
            rstd = kb.sb([128, 1], F32, pc)
            hb = kb.sb([128, D], BF16, pc)
            hT = [kb.sb([128, 8, 128], BF16, pc) for _ in range(2)]
            sq = kb.sb([128, 512], F32, pc)
            ssq = kb.sb([128, 8], F32, pc)
            sdq = kb.sb([128, 8], F32, pc)
            rq = kb.sb([128, 8], F32, pc)
            qn = kb.sb([128, 512], F32, pc)
            qb = kb.sb([128, 512], BF16, pc)
            kib = kb.sb([128, 128], BF16, pc)
            kin = kb.sb([128, 64], F32, pc)
            rt = [kb.sb([128, 64], F32, pc) for _ in range(4)]
            xcs = kb.sb([128, 512], F32, pc)
            uext = [kb.sb([128, 4, 130], F32, pc) for _ in range(2)]
            ytmp = kb.sb([128, 4, 128], F32, pc)
            stg = [kb.sb([128, 512], BF16, pc) for _ in range(6)]
            stg_ds = [kb.ds() for _ in range(6)]
            kb.I(pool, [], [uext[0]], lambda: G.memset(uext[0].t[:], 0.0))
            kb.I(pool, [], [uext[1]], lambda: G.memset(uext[1].t[:], 0.0))

            def rope(src3, dst3, tt, nh):
                co = sincos.t[:, tt, 8:16].rearrange("p (o j) -> p o j", o=1).to_broadcast([128, nh, 8])
                si = sincos.t[:, tt, 0:8].rearrange("p (o j) -> p o j", o=1).to_broadcast([128, nh, 8])
                x1 = src3[:, :, 0:8]
                x2 = src3[:, :, 8:16]
                v3 = lambda b: b.t[:, 0:nh * 8].rearrange("p (h j) -> p h j", j=8)
                return co, si, x1, x2, v3

            def do_rope(src_buf, src3, dst_buf, dst3, tt, nh):
                co, si, x1, x2, v3 = rope(src3, dst3, tt, nh)
                kb.I(dve, [src_buf, sincos], [rt[0]], lambda: V.tensor_tensor(out=v3(rt[0]), in0=x1, in1=co, op=ALU.mult))
                kb.I(dve, [src_buf, sincos], [rt[1]], lambda: V.tensor_tensor(out=v3(rt[1]), in0=x2, in1=si, op=ALU.mult))
                kb.I(dve, [src_buf, sincos], [rt[2]], lambda: V.tensor_tensor(out=v3(rt[2]), in0=x2, in1=co, op=ALU.mult))
                kb.I(dve, [src_buf, sincos], [rt[3]], lambda: V.tensor_tensor(out=v3(rt[3]), in0=x1, in1=si, op=ALU.mult))
                kb.I(dve, [rt[0], rt[1], dst_buf], [dst_buf], lambda: V.tensor_tensor(out=dst3[:, :, 0:8], in0=v3(rt[0]), in1=v3(rt[1]), op=ALU.subtract))
                kb.I(dve, [rt[2], rt[3], dst_buf], [dst_buf], lambda: V.tensor_tensor(out=dst3[:, :, 8:16], in0=v3(rt[2]), in1=v3(rt[3]), op=ALU.add))

            ssq17 = kb.sb([128, 17], F32, pc)
            sd17 = kb.sb([128, 17], F32, pc)
            rq17 = kb.sb([128, 17], F32, pc)

            def headstats(bank, o):
                kb.I(dve, [bank], [sq], lambda: V.tensor_tensor(out=sq.t[:], in0=bank.t[:], in1=bank.t[:], op=ALU.mult))
                kb.I(dve, [sq, ssq17], [ssq17], lambda: V.tensor_reduce(out=ssq17.t[:, o:o + 8], in_=sq.t[:].rearrange("p (h d) -> p h d", d=64), axis=AX.X, op=ALU.add))

            def headscale(bank, gbc, o):
                for hh in range(8):
                    cs = slice(hh * 64, (hh + 1) * 64)
                    kb.I(dve, [bank, rq17, gbc], [qn], lambda: V.scalar_tensor_tensor(out=qn.t[:, cs], in0=bank.t[:, cs], scalar=rq17.t[:, o + hh:o + hh + 1], in1=gbc.t[:, cs], op0=ALU.mult, op1=ALU.mult))

            hbb = [hb, kb.sb([128, D], BF16, pc)]
            qraw = kb.sb([128, 512], F32, pc)
            kraw = kb.sb([128, 512], F32, pc)
            qiraw = kb.sb([128, 512], F32, pc)
            kwraw = kb.sb([128, 72], F32, pc)
            bgraw = kb.sb([128, 512], F32, pc)
            kbq = kb.sb([128, 512], BF16, pc)
            qib = kb.sb([128, 512], BF16, pc)

            def stageA(tt):
                ts_ = slice(tt * 128, (tt + 1) * 128)
                xt = xbuf[tt % 2]
                hb_ = hbb[tt % 2]
                kb.dma(act, xds[tt % 2], [], [xt], lambda: nc.scalar.dma_start(out=xt.t[:], in_=x_d[ts_, :]))
                kb.I(act, [xt], [hb_, ss], lambda: A.activation(out=hb_.t[:], in_=xt.t[:], func=AF.Square, accum_out=ss.t[:, 0:1]))
                kb.I(act, [ss, eps], [sd], lambda: A.activation(out=sd.t[:], in_=ss.t[:], func=AF.Sqrt, scale=1.0 / D, bias=eps.t[:, 0:1]))
                kb.I(dve, [sd], [rstd], lambda: V.reciprocal(out=rstd.t[:], in_=sd.t[:]))
                kb.I(dve, [xt, rstd, ada], [xt], lambda: V.scalar_tensor_tensor(out=xt.t[:], in0=xt.t[:], scalar=rstd.t[:, 0:1], in1=ada.t[:, A1], op0=ALU.mult, op1=ALU.mult))
                kb.I(pool, [xt, ada], [hb_], lambda: G.tensor_tensor(out=hb_.t[:], in0=xt.t[:], in1=ada.t[:, SHIFT1], op=ALU.add))
                if tt == 0:
                    tap("h0", hb_.t[:], [hb_])

            col0 = [1536, 2048, 2560, 3072, 3584]
            wid = [512, 512, 512, 512, 72]
            bQ, bK, bV, bQI, bKW = bk[0], bk[1], bk[2], bk[3], bk[4]
            bXC, bBG, bCG = bk[5], bk[6], bk[2]

            def conv_mm(hTt, part, bb):
                for c in range(4):
                    cc = part * 512 + c * 128
                    for kc in range(8):
                        kb.I(pe, [hTt, Wbf], [bb], lambda: T.matmul(bb.t[:, c * 128:(c + 1) * 128], lhsT=Wbf.t[:, kc, cc:cc + 128], rhs=hTt.t[:, kc, :], start=(kc == 0), stop=(kc == 7)))

            def stageB(tt):
                hb_ = hbb[tt % 2]
                hTt = hT[tt % 2]
                for kc in range(8):
                    kb.I(pe, [hb_, ident], [PT], lambda: T.transpose(out=PT.t[:, kc * 128:(kc + 1) * 128], in_=hb_.t[:, kc * 128:(kc + 1) * 128], identity=ident.t[:]))
                kb.I(act, [PT], [hTt], lambda: A.copy(out=hTt.t[:].rearrange("p a b -> p (a b)"), in_=PT.t[:, :]))
                for gi in range(5):
                    for kc in range(8):
                        kb.I(pe, [hTt, Wbf], [bk[gi]], lambda: T.matmul(bk[gi].t[:, 0:wid[gi]], lhsT=hTt.t[:, kc, :], rhs=Wbf.t[:, kc, col0[gi]:col0[gi] + wid[gi]], start=(kc == 0), stop=(kc == 7)))
                conv_mm(hTt, 0, bXC)
                conv_mm(hTt, 1, bBG)
                bv3 = bV.t[:].rearrange("p (a b) -> p a b", b=128)
                kb.I(act, [bV], [Vp_r[tt]], lambda: A.copy(out=Vp.t[:, tt, :, 0:64], in_=bv3[:, :, 0:64]))
                kb.I(act, [bV], [Vp_r[tt]], lambda: A.copy(out=Vp.t[:, tt, :, 96:160], in_=bv3[:, :, 64:128]))
                kb.I(act, [bKW], [kwraw], lambda: A.copy(out=kwraw.t[:], in_=bKW.t[:, 0:72]))
                conv_mm(hTt, 2, bCG)

            def evac(tt):
                kb.I(act, [bQ], [qraw], lambda: A.copy(out=qraw.t[:], in_=bQ.t[:]))
                kb.I(act, [bK], [kraw], lambda: A.copy(out=kraw.t[:], in_=bK.t[:]))
                kb.I(act, [bQI], [qiraw], lambda: A.copy(out=qiraw.t[:], in_=bQI.t[:]))
                kb.I(act, [bXC], [xcs], lambda: A.copy(out=xcs.t[:], in_=bXC.t[:]))
                kb.I(act, [bBG], [bgraw], lambda: A.copy(out=bgraw.t[:], in_=bBG.t[:]))

            qn3 = qn.t[:].rearrange("p (h d) -> p h d", d=64)

            def post_dve(tt):
                ue, un = uext[tt % 2], uext[(tt + 1) % 2]
                kb.I(dve, [bCG, xcs], [ue], lambda: V.tensor_tensor(out=ue.t[:, :, 2:130], in0=bCG.t[:].rearrange("p (a b) -> p a b", b=128), in1=xcs.t[:].rearrange("p (a b) -> p a b", b=128), op=ALU.mult))
                kb.I(dve, [ue], [un], lambda: V.tensor_copy(out=un.t[:, :, 0:2], in_=ue.t[:, :, 128:130]))
                kb.I(pool, [kwraw], [wi_all], lambda: G.tensor_copy(out=wi_all.t[:, tt, :], in_=kwraw.t[:, 64:72]))
                headstats(qraw, 0)
                headstats(kraw, 8)
                kb.I(dve, [kwraw, ssq17], [sq, ssq17], lambda: V.scalar_tensor_tensor(out=sq.t[:, 0:64], in0=kwraw.t[:, 0:64], scalar=1.0, in1=kwraw.t[:, 0:64], op0=ALU.mult, op1=ALU.mult, accum_out=ssq17.t[:, 16:17]))
                kb.I(act, [ssq17, eps], [sd17], lambda: A.activation(out=sd17.t[:], in_=ssq17.t[:], func=AF.Sqrt, scale=1.0 / 64, bias=eps.t[:, 0:1]))
                kb.I(dve, [sd17], [rq17], lambda: V.reciprocal(out=rq17.t[:], in_=sd17.t[:]))
                headscale(qraw, qg_bc, 0)
                kb.I(pool, [qn], [qb], lambda: G.tensor_copy(out=qb.t[:], in_=qn.t[:]))
                do_rope(qn, qn3, qb, qb.t[:].rearrange("p (h d) -> p h d", d=64), tt, 8)
                if tt == 1:
                    tap("q1", qb.t[:], [qb])
                headscale(kraw, kg_bc, 8)
                kb.I(pool, [qn], [kbq], lambda: G.tensor_copy(out=kbq.t[:], in_=qn.t[:]))
                do_rope(qn, qn3, kbq, kbq.t[:].rearrange("p (h d) -> p h d", d=64), tt, 8)
                if tt == 1:
                    tap("k1", kbq.t[:], [kbq])
                kb.I(pool, [qiraw], [qib], lambda: G.tensor_copy(out=qib.t[:], in_=qiraw.t[:]))
                do_rope(qiraw, qiraw.t[:].rearrange("p (h d) -> p h d", d=64), qib, qib.t[:].rearrange("p (h d) -> p h d", d=64), tt, 8)
                kb.I(dve, [kwraw, rq17, kig_bc], [kin], lambda: V.scalar_tensor_tensor(out=kin.t[:], in0=kwraw.t[:, 0:64], scalar=rq17.t[:, 16:17], in1=kig_bc.t[:], op0=ALU.mult, op1=ALU.mult))
                kb.I(pool, [kin], [kib], lambda: G.tensor_copy(out=kib.t[:, 0:64], in_=kin.t[:]))
                do_rope(kin, kin.t[:].rearrange("p (h d) -> p h d", d=64), kib, kib.t[:, 0:64].rearrange("p (h d) -> p h d", d=64), tt, 1)
                kb.I(dve, [kib], [kib], lambda: V.tensor_copy(out=kib.t[:, 64:128], in_=kib.t[:, 0:64]))
                for c in range(4):
                    kb.I(dve, [ue, convw], [ytmp], lambda: V.tensor_scalar(out=ytmp.t[:, c, :], in0=ue.t[:, c, 2:130], scalar1=convw.t[:, c * 3 + 2:c * 3 + 3], scalar2=None, op0=ALU.mult))
                    kb.I(dve, [ue, convw, ytmp], [ytmp], lambda: V.scalar_tensor_tensor(out=ytmp.t[:, c, :], in0=ue.t[:, c, 1:129], scalar=convw.t[:, c * 3 + 1:c * 3 + 2], in1=ytmp.t[:, c, :], op0=ALU.mult, op1=ALU.add))
                    kb.I(dve, [ue, convw, ytmp], [ytmp], lambda: V.scalar_tensor_tensor(out=ytmp.t[:, c, :], in0=ue.t[:, c, 0:128], scalar=convw.t[:, c * 3:c * 3 + 1], in1=ytmp.t[:, c, :], op0=ALU.mult, op1=ALU.add))
                sc_ = stg[4 + tt % 2]
                kb.I(pool, [bgraw, ytmp], [sc_], lambda: G.tensor_tensor(out=sc_.t[:], in0=bgraw.t[:], in1=ytmp.t[:].rearrange("p a b -> p (a b)"), op=ALU.mult))
                kb.dma(sp, stg_ds[4 + tt % 2], [sc_], [], lambda: nc.sync.dma_start(out=CONVT[tt], in_=sc_.t[:]))

            def post_pe(tt):
                ts_ = slice(tt * 128, (tt + 1) * 128)
                sq_, sk_ = stg[tt % 2], stg[2 + tt % 2]
                for c in range(4):
                    kb.I(pe, [qb, ident], [PT], lambda: T.transpose(out=PT.t[:, c * 128:(c + 1) * 128], in_=qb.t[:, c * 128:(c + 1) * 128], identity=ident.t[:]))
                for c in range(4):
                    kb.I(pe, [kbq, ident], [PT], lambda: T.transpose(out=PT.t[:, 512 + c * 128:512 + (c + 1) * 128], in_=kbq.t[:, c * 128:(c + 1) * 128], identity=ident.t[:]))
                kb.I(act, [PT], [sq_], lambda: A.copy(out=sq_.t[:], in_=PT.t[:, 0:512]))
                kb.dma(sp, stg_ds[tt % 2], [sq_], [], lambda: nc.sync.dma_start(out=QT[tt], in_=sq_.t[:]))
                kb.I(act, [PT], [kT_r[tt]], lambda: A.copy(out=kT.t[:, :, ts_], in_=PT.t[:, 512:1024].rearrange("p (a b) -> p a b", b=128)))
                for c in range(4):
                    kb.I(pe, [qib, ident], [PT], lambda: T.transpose(out=PT.t[:, c * 128:(c + 1) * 128], in_=qib.t[:, c * 128:(c + 1) * 128], identity=ident.t[:]))
                kb.I(pe, [kib, ident], [PT], lambda: T.transpose(out=PT.t[:, 512:640], in_=kib.t[:], identity=ident.t[:]))
                kb.I(act, [PT], [sk_], lambda: A.copy(out=sk_.t[:], in_=PT.t[:, 0:512]))
                kb.dma(sp, stg_ds[2 + tt % 2], [sk_], [], lambda: nc.sync.dma_start(out=QIT[tt], in_=sk_.t[:]))
                kb.I(act, [PT], [kiT_r[tt]], lambda: A.copy(out=kiT2.t[:, ts_], in_=PT.t[:, 512:640]))

            stageA(0)
            for tt in range(NT):
                if tt + 1 < NT:
                    stageA(tt + 1)
                stageB(tt)
                evac(tt)
                if tt > 0:
                    post_pe(tt - 1)
                post_dve(tt)
            post_pe(NT - 1)
            tap("kT", kT.t[:, 0, 0:512], kT_r[0:4])
            tap("kiT", kiT2.t[:, 0:512], kiT_r[0:4])
            tap("wi", wi_all.t[:], [wi_all])
            kb.barrier()


        wb_state = {"e": 0}
        wbstage = []
        wbds = []

        def wb_step():
            e = wb_state["e"]
            if e >= NE:
                return
            wb_state["e"] = e + 1
            stg_ = wbstage[e % 2]
            dsl, dss_ = wbds[e % 2]
            kb.dma(pool, dsl, [], [stg_], lambda: G.dma_start(out=stg_.t[:, 0:2048].rearrange("p (c f) -> p c f", f=256), in_=w1_d[e].rearrange("(c p) f -> p c f", p=128)))
            kb.dma(pool, dsl, [stg_], [stg_], lambda: G.dma_start(out=stg_.t[:, 2048:4096].rearrange("p (c f) -> p c f", f=256), in_=w3_d[e].rearrange("(c p) f -> p c f", p=128)))
            kb.dma(pool, dsl, [stg_], [stg_], lambda: G.dma_start(out=stg_.t[:, 4096:6144].rearrange("p (c f) -> p c f", f=1024), in_=w2_d[e].rearrange("(c p) f -> p c f", p=128)))
            kb.dma(sp, dss_, [stg_], [], lambda: nc.sync.dma_start(out=WB13[e * 128:(e + 1) * 128, :], in_=stg_.t[:, 0:4096]))
            kb.dma(sp, dss_, [stg_], [], lambda: nc.sync.dma_start(out=WB2[e * 128:(e + 1) * 128, :], in_=stg_.t[:, 4096:6144]))

        if upto("D"):
          with ExitStack() as pd:
            wbstage.extend([kb.sb([128, 6144], BF16, pd) for _ in range(2)])
            wbds.extend([(kb.ds(), kb.ds()) for _ in range(2)])
            scoreb = [kb.sb([128, S], F32, pd) for _ in range(2)]
            mbb = [kb.sb([128, S], BF16, pd) for _ in range(2)]
            mTb = [kb.sb([128, S], BF16, pd) for _ in range(2)]
            dw = kb.sb([128, 8, 128], BF16, pd)
            qTb = [kb.sb([128, 512], BF16, pd) for _ in range(2)]
            qiTb = [kb.sb([128, 512], BF16, pd) for _ in range(2)]
            qds = [kb.ds() for _ in range(4)]
            Rb = [kb.sb([128, 512], BF16, pd) for _ in range(4)]
            pTb = [kb.sb([128, 512], BF16, pd) for _ in range(4)]
            lo = kb.sb([128, 1], F32, pd)
            hi = kb.sb([128, 1], F32, pd)
            w0 = kb.sb([128, 1], F32, pd)
            mid = kb.sb([128, 1], F32, pd)
            cnt = kb.sb([128, 1], F32, pd)
            tq = kb.sb([128, 1], F32, pd)
            rs = kb.sb([128, 512], F32, pd)
            bcs = kb.sb([128, 512], F32, pd)
            aob = [kb.sb([128, 512], BF16, pd) for _ in range(2)]
            aods = [kb.ds(), kb.ds()]
            kb.I(pool, [], [rs], lambda: G.memset(rs.t[:], 1.0))
            banksA = [bk[0], bk[1], bk[3], bk[4]]
            negbig = kb.sb([128, 1], F32, pd)
            kb.I(pool, [], [negbig], lambda: G.memset(negbig.t[:], -256.0))
            ident2k = kb.sb([128, 128], BF16, pd)
            kb.I(pool, [ident], [ident2k], lambda: G.tensor_scalar(out=ident2k.t[:], in0=ident.t[:], scalar1=2048.0, scalar2=1.0, op0=ALU.mult, op1=ALU.mult))
            psS = bk[2]
            STb = [bk[3], bk[4], bk[0], bk[1]]
            bankE, bankO = bk[5], bk[6]

            def load_qt(qt):
                a = qTb[qt % 2]
                kb.dma(act, qds[qt % 2], [], [a], lambda: nc.scalar.dma_start(out=a.t[:], in_=QT[qt]))

            def load_qi(qt):
                b_ = qiTb[qt % 2]
                kb.dma(act, qds[2 + qt % 2], [], [b_], lambda: nc.scalar.dma_start(out=b_.t[:], in_=QIT[qt]))

            def idx_phase(qt):
                qiT = qiTb[qt % 2]
                score = scoreb[qt % 2]
                Nk = 128 * (qt + 1)
                for h in range(8):
                    kb.I(pool, [ident, wi_all], [dw], lambda: G.tensor_scalar(out=dw.t[:, h, :], in0=ident.t[:], scalar1=wi_all.t[:, qt, h:h + 1], scalar2=1.0, op0=ALU.mult, op1=ALU.mult))
                wb_step()
                wb_step()
                nch = (Nk + 511) // 512
                for c_ in range(nch):
                    w = min(512, Nk - c_ * 512)
                    tiles = list(range(c_ * 4, c_ * 4 + w // 128))
                    krs = [kiT_r[t] for t in tiles]

                    def mmA(h):
                        hp, base = h // 2, 64 * (h % 2)
                        pa = banksA[h % 4]
                        kb.I(pe, [qiT] + krs, [pa], lambda: T.matmul(pa.t[:, 0:w], lhsT=qiT.t[base:base + 64, hp * 128:(hp + 1) * 128], rhs=kiT2.t[base:base + 64, c_ * 512:c_ * 512 + w], start=True, stop=True))
                    mmA(0)
                    mmA(1)
                    for p_ in range(4):
                        if p_ + 1 < 4:
                            mmA(2 * p_ + 2)
                            mmA(2 * p_ + 3)
                        for h in (2 * p_, 2 * p_ + 1):
                            pa = banksA[h % 4]
                            R = Rb[h % 4]
                            kb.I(act, [pa], [R], lambda: A.activation(out=R.t[:, 0:w], in_=pa.t[:, 0:w], func=AF.Relu))
                        for h in (2 * p_, 2 * p_ + 1):
                            R = Rb[h % 4]
                            kb.I(pe, [dw, R], [psS], lambda: T.matmul(psS.t[:, 0:w], lhsT=dw.t[:, h, :], rhs=R.t[:, 0:w], start=(h == 0), stop=(h == 7 and c_ != nch - 1)))
                    o0 = c_ * 512
                    if c_ == nch - 1:
                        kb.I(pe, [ident, cbias], [psS], lambda: T.matmul(psS.t[:, w - 128:w], lhsT=ident.t[:], rhs=cbias.t[:], start=False, stop=True))
                    kb.I(act, [psS], [score], lambda: A.copy(out=score.t[:, o0:o0 + w], in_=psS.t[:, 0:w]))

            def thr_phase(qt):
                Nk = 128 * (qt + 1)
                mbq = mbb[qt % 2]
                score = scoreb[qt % 2]
                if qt < 2:
                    kb.I(dve, [], [lo], lambda: V.memset(lo.t[:], -1e29))
                else:
                    n0 = qt * 128
                    kb.I(dve, [score], [hi], lambda: V.tensor_reduce(out=hi.t[:], in_=score.t[:, 0:Nk], axis=AX.X, op=ALU.max))
                    kb.I(dve, [score], [lo], lambda: V.tensor_reduce(out=lo.t[:], in_=score.t[:, 0:n0], axis=AX.X, op=ALU.min))
                    kb.I(dve, [lo], [lo], lambda: V.tensor_scalar(out=lo.t[:], in0=lo.t[:], scalar1=-1.0, scalar2=None, op0=ALU.add))
                    kb.I(dve, [hi, lo], [w0], lambda: V.tensor_tensor(out=w0.t[:], in0=hi.t[:], in1=lo.t[:], op=ALU.subtract))
                    for i in range(NIT):
                        f = 2.0 ** -(i + 1)
                        kb.I(dve, [w0, lo], [mid], lambda: V.scalar_tensor_tensor(out=mid.t[:], in0=w0.t[:], scalar=f, in1=lo.t[:], op0=ALU.mult, op1=ALU.add))
                        kb.I(dve, [score, mid], [mbq, cnt], lambda: V.tensor_scalar(out=mbq.t[:, 0:Nk], in0=score.t[:, 0:Nk], scalar1=mid.t[:, 0:1], scalar2=None, op0=ALU.is_gt, op1=ALU.add, accum_out=cnt.t[:, 0:1]))
                        kb.I(dve, [cnt], [tq], lambda: V.tensor_scalar(out=tq.t[:], in0=cnt.t[:], scalar1=NSEL - 0.5, scalar2=-1e30, op0=ALU.is_lt, op1=ALU.mult))
                        kb.I(dve, [tq, mid, lo], [lo], lambda: V.scalar_tensor_tensor(out=lo.t[:], in0=tq.t[:], scalar=mid.t[:, 0:1], in1=lo.t[:], op0=ALU.add, op1=ALU.max))
                kb.I(dve, [score, lo], [mbq], lambda: V.tensor_scalar(out=mbq.t[:, 0:Nk], in0=score.t[:, 0:Nk], scalar1=lo.t[:, 0:1], scalar2=None, op0=ALU.is_gt))

            def maskT_phase(qt):
                mbq, mT = mbb[qt % 2], mTb[qt % 2]
                nsb = qt + 1
                for g0 in range(0, nsb, 8):
                    n = min(8, nsb - g0)
                    for j in range(n):
                        sb_ = g0 + j
                        kb.I(pe, [mbq, ident], [PT], lambda: T.transpose(out=PT.t[:, j * 128:(j + 1) * 128], in_=mbq.t[:, sb_ * 128:(sb_ + 1) * 128], identity=ident.t[:]))
                    kb.I(act, [PT], [mT], lambda: A.copy(out=mT.t[:, g0 * 128:(g0 + n) * 128], in_=PT.t[:, 0:n * 128]))

            def attn_phase(qt):
                qTt = qTb[qt % 2]
                mT = mTb[qt % 2]
                mbq = mbb[qt % 2]
                nsb = qt + 1
                pairs = []
                for hp in range(4):
                    for g in range((nsb + 3) // 4):
                        pairs.append((hp, list(range(4 * g, min(4 * g + 4, nsb)))))

                def qkm(pi):
                    hp, sbs = pairs[pi]
                    pem = (pi % 3 == 0)
                    for j, sb_ in enumerate(sbs):
                        for par in range(2):
                            st = STb[(2 * pi + par) % 4]
                            base = 64 * par
                            kb.I(pe, [kT_r[sb_], qTt], [st], lambda: T.matmul(st.t[:, j * 128:(j + 1) * 128], lhsT=kT.t[base:base + 64, hp, sb_ * 128:(sb_ + 1) * 128], rhs=qTt.t[base:base + 64, hp * 128:(hp + 1) * 128], start=True, stop=not pem))
                        if pem:
                            for par in range(2):
                                st = STb[(2 * pi + par) % 4]
                                kb.I(pe, [mbq, ident2k], [st], lambda: T.matmul(st.t[:, j * 128:(j + 1) * 128], lhsT=mbq.t[:, sb_ * 128:(sb_ + 1) * 128], rhs=ident2k.t[:], start=False, stop=True))
                qkm(0)
                for pi, (hp, sbs) in enumerate(pairs):
                    if pi + 1 < len(pairs):
                        qkm(pi + 1)
                    pem = (pi % 3 == 0)
                    w = len(sbs) * 128
                    c0 = sbs[0] * 128
                    for par in range(2):
                        st = STb[(2 * pi + par) % 4]
                        pt = pTb[(2 * pi + par) % 4]
                        if pem:
                            kb.I(act, [st, negbig], [pt], lambda: A.activation(out=pt.t[:, 0:w], in_=st.t[:, 0:w], func=AF.Exp, scale=0.125, bias=negbig.t[:, 0:1]))
                        else:
                            kb.I(act, [st], [pt], lambda: A.activation(out=pt.t[:, 0:w], in_=st.t[:, 0:w], func=AF.Exp, scale=0.125))
                            kb.I(pool, [pt, mT], [pt], lambda: G.tensor_tensor(out=pt.t[:, 0:w], in0=pt.t[:, 0:w], in1=mT.t[:, c0:c0 + w], op=ALU.mult))
                    for par in range(2):
                        pt = pTb[(2 * pi + par) % 4]
                        for j, sb_ in enumerate(sbs):
                            if par == 0:
                                kb.I(pe, [Vp_r[sb_], pt], [bankE], lambda: T.matmul(bankE.t[0:65, hp * 128:(hp + 1) * 128], lhsT=Vp.t[:, sb_, hp, 0:65], rhs=pt.t[:, j * 128:(j + 1) * 128], start=(sb_ == 0), stop=(sb_ == nsb - 1)))
                            else:
                                kb.I(pe, [Vp_r[sb_], pt], [bankO], lambda: T.matmul(bankO.t[:, hp * 128:(hp + 1) * 128], lhsT=Vp.t[:, sb_, hp, 32:160], rhs=pt.t[:, j * 128:(j + 1) * 128], start=(sb_ == 0), stop=(sb_ == nsb - 1)))
                kb.I(dve, [bankE], [rs], lambda: V.reciprocal(out=rs.t[64:65, :], in_=bankE.t[64:65, :]))
                kb.I(dve, [bankO], [rs], lambda: V.reciprocal(out=rs.t[32:33, :], in_=bankO.t[32:33, :]))
                kb.I(pe, [rs, ones_f], [psS], lambda: T.matmul(psS.t[0:64, :], lhsT=ones_f.t[64:65, 0:64], rhs=rs.t[64:65, :], start=True, stop=True))
                kb.I(pe, [rs, ones_f], [psS], lambda: T.matmul(psS.t[64:128, :], lhsT=ones_f.t[32:33, 0:64], rhs=rs.t[32:33, :], start=True, stop=True))
                kb.I(act, [psS], [bcs], lambda: A.copy(out=bcs.t[:], in_=psS.t[:]))
                ao = aob[qt % 2]
                kb.I(dve, [bankE, bcs], [ao], lambda: V.tensor_tensor(out=ao.t[0:64, :], in0=bankE.t[0:64, :], in1=bcs.t[0:64, :], op=ALU.mult))
                kb.I(dve, [bankO, bcs, ao], [ao], lambda: V.tensor_tensor(out=ao.t[64:128, :], in0=bankO.t[64:128, :], in1=bcs.t[64:128, :], op=ALU.mult))
                kb.dma(sp, aods[qt % 2], [ao], [], lambda: nc.sync.dma_start(out=ATTNT[qt], in_=ao.t[:]))

            load_qi(0)
            load_qt(0)
            idx_phase(0)
            thr_phase(0)
            load_qi(1)
            idx_phase(1)
            for qt in range(NT):
                if qt + 1 < NT:
                    thr_phase(qt + 1)
                    load_qt(qt + 1)
                if qt + 2 < NT:
                    load_qi(qt + 2)
                    idx_phase(qt + 2)
                maskT_phase(qt)
                attn_phase(qt)
            kb.barrier()
            for tn in ("attn0", "attn5"):
                pass
            if "attnT" in tap_d:
                d_ = kb.ds()
                kb.dma(sp, d_, [], [], lambda: nc.sync.dma_start(out=tap_d["attnT"], in_=ATTNT))
            kb.barrier()


        kb.barrier()
        kside.close()
        if upto("E"):
          g_all = kb.sb([128, NT, NE], F32)
          pos_all = kb.sb([128, NT, NE], F32)
          with ExitStack() as pe_:
            adaE = kb.sb([128, 4 * D], F32, pe_)
            GATE1, SHIFT2, A2, GATE2 = [slice(i * D, (i + 1) * D) for i in range(4)]
            eds = kb.ds()
            kb.dma(sp, kb.ds(), [], [adaE], lambda: nc.sync.dma_start(out=adaE.t[:], in_=ADA[:, 2 * D:6 * D]))
            Wout = kb.sb([128, 8, D], BF16, pe_)
            Ws13 = kb.sb([128, 8, 512], BF16, pe_)
            Ws2 = kb.sb([128, 2, D], BF16, pe_)
            Wr = kb.sb([128, 8, NE], BF16, pe_)
            kb.dma(pool, kb.ds(), [], [Wout], lambda: G.dma_start(out=Wout.t[:], in_=wout_d.rearrange("(c p) f -> p c f", p=128)))
            kb.dma(pool, kb.ds(), [], [Wr], lambda: G.dma_start(out=Wr.t[:], in_=wr_d.rearrange("(c p) f -> p c f", p=128)))
            xbuf = [kb.sb([128, D], F32, pe_) for _ in range(2)]
            catb = [kb.sb([128, 8, 128], BF16, pe_) for _ in range(2)]
            lds = [kb.ds() for _ in range(2)]
            ldsx = [kb.ds() for _ in range(2)]
            tmp = kb.sb([128, D], F32, pe_)
            x1 = kb.sb([128, D], F32, pe_)
            junk = kb.sb([128, D], BF16, pe_)
            hf = kb.sb([128, D], F32, pe_)
            h2b = [kb.sb([128, D], BF16, pe_) for _ in range(2)]
            h2ds = [kb.ds(), kb.ds()]
            h2T = kb.sb([128, 8, 128], BF16, pe_)
            baseb = [kb.sb([128, D], F32, pe_) for _ in range(2)]
            bds = [kb.ds(), kb.ds()]
            ss = kb.sb([128, 1], F32, pe_)
            sd = kb.sb([128, 1], F32, pe_)
            rstd = kb.sb([128, 1], F32, pe_)
            scr = kb.sb([128, NE], F32, pe_)
            sel = kb.sb([128, NE], F32, pe_)
            m8 = kb.sb([128, 8], F32, pe_)
            msk = kb.sb([128, NE], F32, pe_)
            mskb = kb.sb([128, NE], BF16, pe_)
            gsel = kb.sb([128, NE], F32, pe_)
            gsum = kb.sb([128, 1], F32, pe_)
            rg = kb.sb([128, 1], F32, pe_)
            sgs = kb.sb([128, 256], F32, pe_)
            t1s = kb.sb([128, 256], F32, pe_)
            aT = kb.sb([128, 256], BF16, pe_)
            x1b = [x1, kb.sb([128, D], F32, pe_), kb.sb([128, D], F32, pe_)]
            h2Tb = [h2T, kb.sb([128, 8, 128], BF16, pe_)]
            tmp2 = kb.sb([128, D], F32, pe_)

            def stage1a(tt):
                x1_ = x1b[tt % 3]
                ts_ = slice(tt * 128, (tt + 1) * 128)
                xt, cat = xbuf[tt % 2], catb[tt % 2]
                kb.dma(act, ldsx[tt % 2], [], [xt], lambda: nc.scalar.dma_start(out=xt.t[:], in_=x_d[ts_, :]))
                kb.dma(act, lds[tt % 2], [], [cat], lambda: nc.scalar.dma_start(out=cat.t[:, 0:4, :], in_=CONVT[tt].rearrange("p (c t) -> p c t", t=128)))
                kb.dma(act, lds[tt % 2], [cat], [cat], lambda: nc.scalar.dma_start(out=cat.t[:, 4:8, :], in_=ATTNT[tt].rearrange("p (c t) -> p c t", t=128)))
                for hf_ in range(2):
                    for kc in range(8):
                        kb.I(pe, [cat, Wout], [bk[hf_]], lambda: T.matmul(bk[hf_].t[:], lhsT=cat.t[:, kc, :], rhs=Wout.t[:, kc, hf_ * 512:(hf_ + 1) * 512], start=(kc == 0), stop=(kc == 7)))
                for hf_ in range(2):
                    cs = slice(hf_ * 512, (hf_ + 1) * 512)
                    kb.I(dve, [bk[hf_], adaE], [tmp], lambda: V.tensor_tensor(out=tmp.t[:, cs], in0=bk[hf_].t[:], in1=adaE.t[:, hf_ * 512:(hf_ + 1) * 512], op=ALU.mult))
                kb.I(pool, [tmp, xt], [x1_], lambda: G.tensor_tensor(out=x1_.t[:], in0=tmp.t[:], in1=xt.t[:], op=ALU.add))
                if tt == 1:
                    tap("x1_1", x1_.t[:], [x1_])
                kb.I(act, [x1_], [junk, ss], lambda: A.activation(out=junk.t[:], in_=x1_.t[:], func=AF.Square, accum_out=ss.t[:, 0:1]))
                kb.I(act, [ss, eps], [sd], lambda: A.activation(out=sd.t[:], in_=ss.t[:], func=AF.Ln, scale=1.0 / D, bias=eps.t[:, 0:1]))
                kb.I(act, [sd], [rstd], lambda: A.activation(out=rstd.t[:], in_=sd.t[:], func=AF.Exp, scale=-0.5))
                kb.I(dve, [x1_, rstd, adaE], [hf], lambda: V.scalar_tensor_tensor(out=hf.t[:], in0=x1_.t[:], scalar=rstd.t[:, 0:1], in1=adaE.t[:, A2], op0=ALU.mult, op1=ALU.mult))
                hb2 = h2b[tt % 2]
                kb.I(pool, [hf, adaE], [hb2], lambda: G.tensor_tensor(out=hb2.t[:], in0=hf.t[:], in1=adaE.t[:, SHIFT2], op=ALU.add))
                kb.dma(sp, h2ds[tt % 2], [hb2], [], lambda: nc.sync.dma_start(out=H2[ts_, :], in_=hb2.t[:]))

            def stage1b(tt):
                hb2 = h2b[tt % 2]
                h2T_ = h2Tb[tt % 2]
                for kc in range(8):
                    kb.I(pe, [hb2, ident], [PT], lambda: T.transpose(out=PT.t[:, kc * 128:(kc + 1) * 128], in_=hb2.t[:, kc * 128:(kc + 1) * 128], identity=ident.t[:]))
                kb.I(act, [PT], [h2T_], lambda: A.copy(out=h2T_.t[:].rearrange("p a b -> p (a b)"), in_=PT.t[:, :]))

            def stage2(tt):
                ts_ = slice(tt * 128, (tt + 1) * 128)
                x1_ = x1b[tt % 3]
                h2T_ = h2Tb[tt % 2]
                for kc in range(8):
                    kb.I(pe, [h2T_, Wr], [bk[2]], lambda: T.matmul(bk[2].t[:, 0:NE], lhsT=h2T_.t[:, kc, :], rhs=Wr.t[:, kc, :], start=(kc == 0), stop=(kc == 7)))
                kb.I(act, [bk[2]], [scr], lambda: A.activation(out=scr.t[:], in_=bk[2].t[:, 0:NE], func=AF.Exp, scale=-1.0))
                kb.I(dve, [scr], [scr], lambda: V.tensor_scalar(out=scr.t[:], in0=scr.t[:], scalar1=1.0, scalar2=None, op0=ALU.add))
                kb.I(dve, [scr], [scr], lambda: V.reciprocal(out=scr.t[:], in_=scr.t[:]))
                kb.I(dve, [scr, rb_bc], [sel], lambda: V.tensor_tensor(out=sel.t[:], in0=scr.t[:], in1=rb_bc.t[:], op=ALU.add))
                kb.I(dve, [sel], [m8], lambda: V.max(out=m8.t[:], in_=sel.t[:]))
                kb.I(dve, [sel, m8], [msk], lambda: V.tensor_scalar(out=msk.t[:], in0=sel.t[:], scalar1=m8.t[:, 7:8], scalar2=None, op0=ALU.is_ge))
                kb.I(dve, [msk], [mskb], lambda: V.tensor_copy(out=mskb.t[:], in_=msk.t[:]))
                kb.I(pe, [ustrict, mskb], [bk[3]], lambda: T.matmul(bk[3].t[:, 0:NE], lhsT=ustrict.t[:], rhs=mskb.t[:], start=True, stop=True))
                kb.I(pe, [ones_bf, mskb], [bk[3]], lambda: T.matmul(bk[3].t[:, NE:2 * NE], lhsT=ones_bf.t[:], rhs=mskb.t[:], start=True, stop=True))
                kb.I(dve, [msk, scr], [gsel, gsum], lambda: V.scalar_tensor_tensor(out=gsel.t[:], in0=msk.t[:], scalar=1.0, in1=scr.t[:], op0=ALU.mult, op1=ALU.mult, accum_out=gsum.t[:, 0:1]))
                kb.I(dve, [gsum], [rg], lambda: V.reciprocal(out=rg.t[:], in_=gsum.t[:]))
                kb.I(dve, [gsel, rg], [g_all], lambda: V.tensor_scalar(out=g_all.t[:, tt, :], in0=gsel.t[:], scalar1=rg.t[:, 0:1], scalar2=2.5, op0=ALU.mult, op1=ALU.mult))
                kb.I(dve, [bk[3], cum_bc], [pos_all], lambda: V.tensor_tensor(out=pos_all.t[:, tt, :], in0=bk[3].t[:, 0:NE], in1=cum_bc.t[:], op=ALU.add))
                kb.I(dve, [bk[3], cum_bc], [cum_bc], lambda: V.tensor_tensor(out=cum_bc.t[:], in0=bk[3].t[:, NE:2 * NE], in1=cum_bc.t[:], op=ALU.add))
                kb.dma(sp, bds[tt % 2], [x1_], [], lambda: nc.sync.dma_start(out=BASE[ts_, :], in_=x1_.t[:]))

            stage1a(0)
            stage1a(1)
            stage1b(0)
            for tt in range(NT):
                if tt + 2 < NT:
                    stage1a(tt + 2)
                if tt + 1 < NT:
                    stage1b(tt + 1)
                stage2(tt)
            tap("g_all", g_all.t[:], [g_all])
            tap("pos_all", pos_all.t[:], [pos_all])
            kb.barrier()

        if upto("F"):
          with ExitStack() as p2:
            cnti = kb.sb([128, NE], I32, p2)
            padf = kb.sb([128, NE], F32, p2)
            cs_a = kb.sb([128, NE], F32, p2)
            cs_b = kb.sb([128, NE], F32, p2)
            pstart = kb.sb([128, NE], F32, p2)
            slotm = kb.sb([128, NE], F32, p2)
            mk_ = kb.sb([128, NE], F32, p2)
            s8f = kb.sb([128, 8], F32, p2)
            jk = kb.sb([128, NE], F32, p2)
            bef = kb.sb([128, NB], F32, p2)
            bstart = kb.sb([128, NB], F32, p2)
            skipf = kb.sb([128, NB], F32, p2)
            same2 = kb.sb([128, NB], F32, p2)
            zl = kb.sb([128, NSLOT // 128], I32, p2)
            li_a = kb.sb([128, 8], I32, p2)
            li_b = kb.sb([128, 8], I32, p2)
            lf_a = kb.sb([128, 8], F32, p2)
            lf_b = kb.sb([128, 8], F32, p2)
            li_all = kb.sb([128, NT, 8], I32, p2)
            lds_ = kb.ds()
            lds2_ = kb.ds()
            kb.I(pool, [], [zl], lambda: G.memset(zl.t[:], 1048576))
            list_r = Res()
            kb.dma(sp, lds_, [zl], [list_r], lambda: nc.sync.dma_start(out=LIST.rearrange("(p b) o -> p (b o)", p=128), in_=zl.t[:]))
            kb.I(dve, [cum_bc], [padf], lambda: V.tensor_scalar(out=padf.t[:], in0=cum_bc.t[:], scalar1=float(BS - 1), scalar2=None, op0=ALU.add))
            kb.I(dve, [padf], [cnti], lambda: V.tensor_copy(out=cnti.t[:], in_=padf.t[:]))
            kb.I(dve, [cnti], [cnti], lambda: V.tensor_scalar(out=cnti.t[:], in0=cnti.t[:], scalar1=8, scalar2=None, op0=ALU.arith_shift_right))
            kb.I(dve, [cnti], [cnti], lambda: V.tensor_scalar(out=cnti.t[:], in0=cnti.t[:], scalar1=8, scalar2=None, op0=ALU.logical_shift_left))
            kb.I(dve, [cnti], [padf], lambda: V.tensor_copy(out=padf.t[:], in_=cnti.t[:]))
            kb.I(dve, [padf], [cs_a], lambda: V.tensor_copy(out=cs_a.t[:], in_=padf.t[:]))
            cur, nxt = cs_a, cs_b
            sh = 1
            while sh < NE:
                kb.I(dve, [cur], [nxt], lambda: V.tensor_copy(out=nxt.t[:, 0:sh], in_=cur.t[:, 0:sh]))
                kb.I(dve, [cur, nxt], [nxt], lambda: V.tensor_tensor(out=nxt.t[:, sh:NE], in0=cur.t[:, sh:NE], in1=cur.t[:, 0:NE - sh], op=ALU.add))
                cur, nxt = nxt, cur
                sh *= 2
            pend = cur
            kb.I(dve, [pend, padf], [pstart], lambda: V.tensor_tensor(out=pstart.t[:], in0=pend.t[:], in1=padf.t[:], op=ALU.subtract))
            tap("pend", pend.t[0:1, :], [pend])
            Ws13 = kb.sb([128, 8, 512], BF16, p2)
            Ws2 = kb.sb([128, 2, D], BF16, p2)
            gate2s = kb.sb([128, D], F32, p2)
            sds = [kb.ds() for _ in range(3)]
            kb.dma(pool, sds[0], [], [Ws13], lambda: G.dma_start(out=Ws13.t[:, :, 0:256], in_=ws1_d.rearrange("(c p) f -> p c f", p=128)))
            kb.dma(pool, sds[0], [Ws13], [Ws13], lambda: G.dma_start(out=Ws13.t[:, :, 256:512], in_=ws3_d.rearrange("(c p) f -> p c f", p=128)))
            kb.dma(pool, sds[1], [], [Ws2], lambda: G.dma_start(out=Ws2.t[:], in_=ws2_d.rearrange("(c p) f -> p c f", p=128)))
            kb.dma(sp, sds[2], [], [gate2s], lambda: nc.sync.dma_start(out=gate2s.t[:], in_=ADA[:, 5 * D:6 * D]))
            shb = [kb.sb([128, D], BF16, p2) for _ in range(2)]
            sx1 = [kb.sb([128, D], F32, p2) for _ in range(2)]
            sld = [kb.ds(), kb.ds()]
            sld2 = [kb.ds(), kb.ds()]
            sh2T = kb.sb([128, 8, 128], BF16, p2)
            ssg = kb.sb([128, 256], F32, p2)
            st1 = kb.sb([128, 256], F32, p2)
            saT = kb.sb([128, 256], BF16, p2)
            stmp = kb.sb([128, D], F32, p2)
            sbase = [kb.sb([128, D], F32, p2) for _ in range(2)]
            sbd = [kb.ds(), kb.ds()]

            def sh_load(tt):
                ts_ = slice(tt * 128, (tt + 1) * 128)
                kb.dma(act, sld[tt % 2], [], [shb[tt % 2]], lambda: nc.scalar.dma_start(out=shb[tt % 2].t[:], in_=H2[ts_, :]))
                kb.dma(act, sld2[tt % 2], [], [sx1[tt % 2]], lambda: nc.scalar.dma_start(out=sx1[tt % 2].t[:], in_=BASE[ts_, :]))

            def sh_tile(tt):
                ts_ = slice(tt * 128, (tt + 1) * 128)
                hb_, x1_ = shb[tt % 2], sx1[tt % 2]
                for kc in range(8):
                    kb.I(pe, [hb_, ident], [PT], lambda: T.transpose(out=PT.t[:, kc * 128:(kc + 1) * 128], in_=hb_.t[:, kc * 128:(kc + 1) * 128], identity=ident.t[:]))
                kb.I(act, [PT], [sh2T], lambda: A.copy(out=sh2T.t[:].rearrange("p a b -> p (a b)"), in_=PT.t[:, :]))
                for j in range(4):
                    for kc in range(8):
                        kb.I(pe, [sh2T, Ws13], [bk[4]], lambda: T.matmul(bk[4].t[:, j * 128:(j + 1) * 128], lhsT=Ws13.t[:, kc, j * 128:(j + 1) * 128], rhs=sh2T.t[:, kc, :], start=(kc == 0), stop=(kc == 7)))
                kb.I(act, [bk[4]], [ssg], lambda: A.activation(out=ssg.t[:], in_=bk[4].t[:, 0:256], func=AF.Exp, scale=-1.0))
                kb.I(dve, [ssg], [ssg], lambda: V.tensor_scalar(out=ssg.t[:], in0=ssg.t[:], scalar1=1.0, scalar2=None, op0=ALU.add))
                kb.I(dve, [ssg], [ssg], lambda: V.reciprocal(out=ssg.t[:], in_=ssg.t[:]))
                kb.I(dve, [bk[4], ssg], [st1], lambda: V.tensor_tensor(out=st1.t[:], in0=bk[4].t[:, 0:256], in1=ssg.t[:], op=ALU.mult))
                kb.I(dve, [bk[4], st1], [saT], lambda: V.tensor_tensor(out=saT.t[:], in0=st1.t[:], in1=bk[4].t[:, 256:512], op=ALU.mult))
                for hf_ in range(2):
                    for fc in range(2):
                        kb.I(pe, [saT, Ws2], [bk[5 + hf_]], lambda: T.matmul(bk[5 + hf_].t[:], lhsT=saT.t[:, fc * 128:(fc + 1) * 128], rhs=Ws2.t[:, fc, hf_ * 512:(hf_ + 1) * 512], start=(fc == 0), stop=(fc == 1)))
                for hf_ in range(2):
                    cs = slice(hf_ * 512, (hf_ + 1) * 512)
                    kb.I(dve, [bk[5 + hf_], gate2s], [stmp], lambda: V.tensor_tensor(out=stmp.t[:, cs], in0=bk[5 + hf_].t[:], in1=gate2s.t[:, cs], op=ALU.mult))
                bs_ = sbase[tt % 2]
                kb.I(pool, [stmp, x1_], [bs_], lambda: G.tensor_tensor(out=bs_.t[:], in0=stmp.t[:], in1=x1_.t[:], op=ALU.add))
                kb.dma(sp, sbd[tt % 2], [bs_], [], lambda: nc.sync.dma_start(out=BASE[ts_, :], in_=bs_.t[:]))

            li_r = [Res() for _ in range(NT)]

            def e2_dve(tt):
                kb.I(dve, [g_all], [mk_], lambda: V.tensor_scalar(out=mk_.t[:], in0=g_all.t[:, tt, :], scalar1=0.0, scalar2=None, op0=ALU.is_gt))
                kb.I(dve, [pos_all, pstart], [slotm], lambda: V.scalar_tensor_tensor(out=slotm.t[:], in0=pos_all.t[:, tt, :], scalar=1.0, in1=pstart.t[:], op0=ALU.add, op1=ALU.add))
                kb.I(dve, [slotm, mk_], [slotm], lambda: V.tensor_tensor(out=slotm.t[:], in0=slotm.t[:], in1=mk_.t[:], op=ALU.mult))
                kb.I(dve, [slotm], [s8f], lambda: V.max(out=s8f.t[:], in_=slotm.t[:]))
                for j in range(8):
                    kb.I(dve, [slotm, s8f, g_all], [jk, g8_all], lambda: V.scalar_tensor_tensor(out=jk.t[:], in0=slotm.t[:], scalar=s8f.t[:, j:j + 1], in1=g_all.t[:, tt, :], op0=ALU.is_equal, op1=ALU.mult, accum_out=g8_all.t[:, tt, j:j + 1]))
                kb.I(dve, [s8f], [s8_all], lambda: V.tensor_scalar(out=s8_all.t[:, tt, :], in0=s8f.t[:], scalar1=-1.0, scalar2=0.0, op0=ALU.add, op1=ALU.max))
                kb.I(dve, [s8_all], [li_a], lambda: V.tensor_scalar(out=li_a.t[:], in0=s8_all.t[:, tt, :], scalar1=7, scalar2=None, op0=ALU.arith_shift_right))
                kb.I(dve, [s8_all], [li_b], lambda: V.tensor_scalar(out=li_b.t[:], in0=s8_all.t[:, tt, :], scalar1=127, scalar2=None, op0=ALU.bitwise_and))
                kb.I(dve, [li_a], [lf_a], lambda: V.tensor_copy(out=lf_a.t[:], in_=li_a.t[:]))
                kb.I(dve, [li_b], [lf_b], lambda: V.tensor_copy(out=lf_b.t[:], in_=li_b.t[:]))
                kb.I(dve, [lf_a, lf_b], [li_r[tt]], lambda: V.scalar_tensor_tensor(out=li_all.t[:, tt, :], in0=lf_b.t[:], scalar=float(NSLOT // 128), in1=lf_a.t[:], op0=ALU.mult, op1=ALU.add))

            def e2_scatter(tt):
                for j in range(8):
                    kb.dma(pool, lds2_, [li_r[tt], tok_all, list_r], [], lambda: G.indirect_dma_start(out=LIST, out_offset=bass.IndirectOffsetOnAxis(ap=li_all.t[:, tt, j:j + 1], axis=0), in_=tok_all.t[:, tt:tt + 1], in_offset=None))

            sh_load(0)
            e2_dve(0)
            for tt in range(NT):
                if tt + 1 < NT:
                    sh_load(tt + 1)
                    e2_dve(tt + 1)
                sh_tile(tt)
                e2_scatter(tt)
            for b in range(NB):
                kb.I(dve, [pend], [jk, bef], lambda: V.tensor_scalar(out=jk.t[:], in0=pend.t[:], scalar1=float(b * BS), scalar2=None, op0=ALU.is_le, op1=ALU.add, accum_out=bef.t[:, b:b + 1]))
            kb.I(pool, [], [bstart], lambda: G.iota(bstart.t[:], pattern=[[BS, NB]], base=0, channel_multiplier=0, allow_small_or_imprecise_dtypes=True))
            kb.I(dve, [bstart, pend], [skipf], lambda: V.tensor_scalar(out=skipf.t[:], in0=bstart.t[:], scalar1=pend.t[:, NE - 1:NE], scalar2=None, op0=ALU.is_ge))
            kb.I(dve, [bef], [same2], lambda: V.memset(same2.t[:], 0.0))
            kb.I(dve, [bef, same2], [same2], lambda: V.tensor_tensor(out=same2.t[:, 2:NB], in0=bef.t[:, 2:NB], in1=bef.t[:, 0:NB - 2], op=ALU.is_equal))
            kb.I(dve, [skipf, same2], [skipf], lambda: V.tensor_tensor(out=skipf.t[:], in0=skipf.t[:], in1=same2.t[:], op=ALU.max))
            kb.I(dve, [bef], [bef], lambda: V.tensor_scalar(out=bef.t[:], in0=bef.t[:], scalar1=float(NE - 1), scalar2=128.0, op0=ALU.min, op1=ALU.mult))
            kb.I(dve, [bef, skipf], [bef], lambda: V.scalar_tensor_tensor(out=bef.t[:], in0=skipf.t[:], scalar=1048576.0, in1=bef.t[:], op0=ALU.mult, op1=ALU.add))
            kb.I(dve, [bef, piota], [widx], lambda: V.tensor_scalar(out=widx.t[:], in0=bef.t[:], scalar1=piota.t[:, 0:1], scalar2=None, op0=ALU.add))
            tap("g8", g8_all.t[:], [g8_all])
            tap("s8", s8_all.t[:], [s8_all])
            tap("widx", widx.t[:], [widx])
            while wb_state["e"] < NE:
                if not wbstage:
                    wbstage.extend([kb.sb([128, 6144], BF16, p2) for _ in range(2)])
                    wbds.extend([(kb.ds(), kb.ds()) for _ in range(2)])
                wb_step()
            kb.barrier()

        if upto("F"):
          with ExitStack() as pf:
            reg_w = G.to_reg(NE * 128 - 1)
            reg_t = G.to_reg(S - 1)
            w13b = [kb.sb([128, 4096], BF16, pf) for _ in range(2)]
            w2b = [kb.sb([128, 2048], BF16, pf) for _ in range(2)]
            wgds = [kb.ds() for _ in range(4)]
            tl_all = kb.sb([128, NSLOT // 128], I32, pf)
            tlds = kb.ds()
            kb.dma(sp, tlds, [], [tl_all], lambda: nc.sync.dma_start(out=tl_all.t[:], in_=LIST.rearrange("(p b) o -> p (b o)", p=128)))
            xg = [kb.sb([128, D], BF16, pf) for _ in range(4)]
            xgds = [kb.ds() for _ in range(4)]
            xgT = [kb.sb([128, 8, 256], BF16, pf) for _ in range(2)]
            for xb_ in xg:
                kb.I(pool, [], [xb_], lambda: G.memset(xb_.t[:], 0.0))
            sg = kb.sb([128, 512], F32, pf)
            t1 = kb.sb([128, 512], F32, pf)
            aT = [kb.sb([128, 512], BF16, pf) for _ in range(2)]
            ysb = [kb.sb([128, D], F32, pf) for _ in range(4)]
            yds = [kb.ds() for _ in range(4)]
            def gath_w13(b):
                wb_ = w13b[b % 2]
                kb.dma(pool, wgds[b % 2], [widx], [wb_], lambda: G.indirect_dma_start(out=wb_.t[:], out_offset=None, in_=WB13, in_offset=bass.IndirectOffsetOnAxis(ap=widx.t[:, b:b + 1], axis=0), bounds_check=reg_w, oob_is_err=False))

            def gath_w2(b):
                wb_ = w2b[b % 2]
                kb.dma(pool, wgds[2 + b % 2], [widx], [wb_], lambda: G.indirect_dma_start(out=wb_.t[:], out_offset=None, in_=WB2, in_offset=bass.IndirectOffsetOnAxis(ap=widx.t[:, b:b + 1], axis=0), bounds_check=reg_w, oob_is_err=False))

            def gath_x(b):
                for sk in range(2):
                    i4 = (2 * b + sk) % 4
                    kb.dma(pool, xgds[i4], [tl_all], [xg[i4]], lambda: G.indirect_dma_start(out=xg[i4].t[:], out_offset=None, in_=H2, in_offset=bass.IndirectOffsetOnAxis(ap=tl_all.t[:, 2 * b + sk:2 * b + sk + 1], axis=0), bounds_check=reg_t, oob_is_err=False))

            PT2 = bk[6].t[:].bitcast(BF16)

            def tr(b):
                xT = xgT[b % 2]
                for sk in range(2):
                    i4 = (2 * b + sk) % 4
                    if sk == 0:
                        for kc in range(8):
                            kb.I(pe, [xg[i4], ident], [PT], lambda: T.transpose(out=PT.t[:, kc * 128:(kc + 1) * 128], in_=xg[i4].t[:, kc * 128:(kc + 1) * 128], identity=ident.t[:]))
                        kb.I(act, [PT], [xT], lambda: A.copy(out=xT.t[:, :, 0:128], in_=PT.t[:, :].rearrange("p (a b) -> p a b", b=128)))
                    else:
                        for kc in range(8):
                            kb.I(pe, [xg[i4], ident], [bk[6]], lambda: T.transpose(out=PT2[:, kc * 128:(kc + 1) * 128], in_=xg[i4].t[:, kc * 128:(kc + 1) * 128], identity=ident.t[:]))
                        kb.I(dve, [bk[6], xT], [xT], lambda: V.tensor_copy(out=xT.t[:, :, 128:256], in_=PT2.rearrange("p (a b) -> p a b", b=128)))

            def hmm(b):
                wb_, xT = w13b[b % 2], xgT[b % 2]
                for m_ in range(2):
                    for fc in range(2):
                        for kc in range(8):
                            kb.I(pe, [wb_, xT], [bk[m_]], lambda: T.matmul(bk[m_].t[:, fc * 256:(fc + 1) * 256], lhsT=wb_.t[:, m_ * 2048 + kc * 256 + fc * 128:m_ * 2048 + kc * 256 + (fc + 1) * 128], rhs=xT.t[:, kc, :], start=(kc == 0), stop=(kc == 7)))

            def silu(b):
                a_ = aT[b % 2]
                kb.I(act, [bk[0]], [sg], lambda: A.activation(out=sg.t[:], in_=bk[0].t[:], func=AF.Sigmoid))
                kb.I(dve, [bk[0], sg], [t1], lambda: V.tensor_tensor(out=t1.t[:], in0=bk[0].t[:], in1=sg.t[:], op=ALU.mult))
                kb.I(dve, [bk[1], t1], [a_], lambda: V.tensor_tensor(out=a_.t[:], in0=t1.t[:], in1=bk[1].t[:], op=ALU.mult))

            def w2(b):
                wb_, a_ = w2b[b % 2], aT[b % 2]
                for sk in range(2):
                    yb = ysb[(2 * b + sk) % 4]
                    for hf_ in range(2):
                        ob = bk[2 + 2 * sk + hf_]
                        for fc in range(2):
                            kb.I(pe, [a_, wb_], [ob], lambda: T.matmul(ob.t[:], lhsT=a_.t[:, fc * 256 + sk * 128:fc * 256 + (sk + 1) * 128], rhs=wb_.t[:, fc * 1024 + hf_ * 512:fc * 1024 + (hf_ + 1) * 512], start=(fc == 0), stop=(fc == 1)))
                    kb.I(act, [bk[2 + 2 * sk]], [yb], lambda: A.copy(out=yb.t[:, 0:512], in_=bk[2 + 2 * sk].t[:]))
                    kb.I(dve, [bk[3 + 2 * sk], yb], [yb], lambda: V.tensor_copy(out=yb.t[:, 512:1024], in_=bk[3 + 2 * sk].t[:]))
                    r0 = b * BS + sk * 128
                    kb.dma(sp, yds[(2 * b + sk) % 4], [yb], [], lambda: nc.sync.dma_start(out=YS[r0:r0 + 128, :], in_=yb.t[:]))

            gath_w13(0)
            gath_x(0)
            gath_w13(1)
            gath_w2(0)
            gath_x(1)
            gath_w2(1)
            tr(0)
            hmm(0)
            gath_w13(2)
            for b in range(NB):
                silu(b)
                if b + 2 < NB:
                    gath_x(b + 2)
                if b + 1 < NB:
                    tr(b + 1)
                w2(b)
                if b + 2 < NB:
                    gath_w2(b + 2)
                if b + 1 < NB:
                    hmm(b + 1)
                if b + 3 < NB:
                    gath_w13(b + 3)
            kb.barrier()

        if upto("G"):
          with ExitStack() as pg:
            gate2 = kb.sb([128, D], F32, pg)
            gds_ = kb.ds()
            kb.dma(sp, gds_, [], [gate2], lambda: nc.sync.dma_start(out=gate2.t[:], in_=ADA[:, 5 * D:6 * D]))
            baseb = [kb.sb([128, D], F32, pg) for _ in range(3)]
            bds = [kb.ds(), kb.ds(), kb.ds()]
            yg = [[kb.sb([128, D], F32, pg) for _ in range(8)] for _ in range(3)]
            ygds = [kb.ds() for _ in range(3)]
            acc = kb.sb([128, D], F32, pg)
            ob_ = [kb.sb([128, D], F32, pg) for _ in range(2)]
            ods = [kb.ds(), kb.ds()]
            def g_load(tt):
                ts_ = slice(tt * 128, (tt + 1) * 128)
                p_, q_ = tt % 2, tt % 3
                kb.dma(sp, bds[q_], [], [baseb[q_]], lambda: nc.sync.dma_start(out=baseb[q_].t[:], in_=BASE[ts_, :]))
                for j in range(8):
                    kb.dma(pool, ygds[q_], [s8_all], [yg[q_][j]], lambda: G.indirect_dma_start(out=yg[q_][j].t[:], out_offset=None, in_=YS, in_offset=bass.IndirectOffsetOnAxis(ap=s8_all.t[:, tt, j:j + 1], axis=0)))
                for j in range(8):
                    yg[q_][j].r.w = yg[q_][7].r.w

            g_load(0)
            g_load(1)
            for tt in range(NT):
                ts_ = slice(tt * 128, (tt + 1) * 128)
                p_, q_ = tt % 2, tt % 3
                if tt + 2 < NT:
                    g_load(tt + 2)
                pb = (bk[0], bk[1]) if tt % 2 == 0 else (bk[2], bk[3])
                for hf_ in range(2):
                    cs = slice(hf_ * 512, (hf_ + 1) * 512)
                    kb.I(dve, [yg[q_][0], g8_all], [pb[hf_]], lambda: V.tensor_scalar(out=pb[hf_].t[:], in0=yg[q_][0].t[:, cs], scalar1=g8_all.t[:, tt, 0:1], scalar2=None, op0=ALU.mult))
                for j in range(1, 8):
                    for hf_ in range(2):
                        cs = slice(hf_ * 512, (hf_ + 1) * 512)
                        kb.I(dve, [yg[q_][j], g8_all, pb[hf_]], [pb[hf_]], lambda: V.scalar_tensor_tensor(out=pb[hf_].t[:], in0=yg[q_][j].t[:, cs], scalar=g8_all.t[:, tt, j:j + 1], in1=pb[hf_].t[:], op0=ALU.mult, op1=ALU.add))
                for hf_ in range(2):
                    cs = slice(hf_ * 512, (hf_ + 1) * 512)
                    kb.I(dve, [pb[hf_], gate2], [acc], lambda: V.tensor_tensor(out=acc.t[:, cs], in0=pb[hf_].t[:], in1=gate2.t[:, cs], op=ALU.mult))
                o_ = ob_[p_]
                kb.I(pool, [acc, baseb[q_]], [o_], lambda: G.tensor_tensor(out=o_.t[:], in0=acc.t[:], in1=baseb[q_].t[:], op=ALU.add))
                kb.dma(sp, ods[p_], [o_], [], lambda: nc.sync.dma_start(out=out_d[ts_, :], in_=o_.t[:]))
            kb.barrier()

        kb.barrier()
    return nc


def _prep_inputs(inputs):
    f = np.float32
    common = {
        "norm1_g": np.ascontiguousarray(inputs["norm1_g"], f).reshape(1, D),
        "norm2_g": np.ascontiguousarray(inputs["norm2_g"], f).reshape(1, D),
        "w_ada": np.ascontiguousarray(inputs["w_ada"][0], f),
        "b_ada": np.ascontiguousarray(inputs["b_ada"], f).reshape(1, 6 * D),
        "w_in": np.ascontiguousarray(inputs["w_in"][0], f),
        "convw": np.ascontiguousarray(inputs["conv_w"][0].reshape(3, 4, 128).transpose(2, 1, 0).reshape(128, 12), f),
        "qg8": np.ascontiguousarray(np.tile(inputs["q_norm_g"][0], 8).reshape(1, 512), f),
        "kg8": np.ascontiguousarray(np.tile(inputs["k_norm_g"][0], 8).reshape(1, 512), f),
        "kig": np.ascontiguousarray(inputs["kidx_norm_g"], f).reshape(1, 64),
        "w_out": np.ascontiguousarray(inputs["w_out"][0], f),
        "w_router": np.ascontiguousarray(inputs["w_router"][0], f),
        "router_bias": np.ascontiguousarray(inputs["router_bias"], f).reshape(1, NE),
        "w1": np.ascontiguousarray(inputs["w1"][0], f),
        "w3": np.ascontiguousarray(inputs["w3"][0], f),
        "w2": np.ascontiguousarray(inputs["w2"][0], f),
        "ws1": np.ascontiguousarray(inputs["ws1"][0], f),
        "ws3": np.ascontiguousarray(inputs["ws3"][0], f),
        "ws2": np.ascontiguousarray(inputs["ws2"][0], f),
    }
    maps = []
    for b in range(8):
        m = dict(common)
        m["x"] = np.ascontiguousarray(inputs["x"][b], f)
        m["cT"] = np.ascontiguousarray(inputs["c"][b].reshape(8, 128).T, f)
        m["posT"] = np.ascontiguousarray(inputs["positions"][b].reshape(NT, 128).T.astype(np.int32))
        maps.append(m)
    return maps


def kernel(**inputs):
    maps = _prep_inputs(inputs)
    nc = build()
    res = run_bass_kernel_spmd(nc, maps, core_ids=list(range(8)))
    return np.stack([np.asarray(r["out"], dtype=np.float32) for r in res.results], axis=0)
```

```python
import math
from contextlib import ExitStack

import numpy as np
import concourse.bass as bass
import concourse.mybir as mybir
from concourse.bass_utils import run_bass_kernel_spmd

F32 = mybir.dt.float32
BF16 = mybir.dt.bfloat16
I32 = mybir.dt.int32
AF = mybir.ActivationFunctionType
ALU = mybir.AluOpType
AX = mybir.AxisListType

S = 4096
D = 1024
NT = S // 128
NE = 64
BS = 256
NB = (S * 8 + NE * BS) // BS
NSLOT = NB * BS
NIT = 14
NSEL = 256
IN_COLS = 3656
TWO_PI = 2.0 * math.pi


class Eng:
    def __init__(self, name, h, sem):
        self.name, self.h, self.sem, self.n, self.seen = name, h, sem, 0, {}


class Res:
    __slots__ = ("w", "r")

    def __init__(self):
        self.w = None
        self.r = {}


class DS:
    def __init__(self, sem):
        self.sem, self.val = sem, 0


class Buf:
    def __init__(self, t):
        self.t = t
        self.r = Res()


class KB:
    def __init__(self, nc, es):
        self.nc, self.es = nc, es
        mk = lambda n, h: Eng(n, h, es.enter_context(nc.semaphore("sem_" + n)))
        self.pe = mk("pe", nc.tensor)
        self.act = mk("act", nc.scalar)
        self.dve = mk("dve", nc.vector)
        self.pool = mk("pool", nc.gpsimd)
        self.sp = mk("sp", nc.sync)
        self.engs = [self.pe, self.act, self.dve, self.pool, self.sp]
        self.dss = []
        self.nbuf = 0

    def ds(self):
        d = DS(self.es.enter_context(self.nc.semaphore("ds%d" % len(self.dss))))
        self.dss.append(d)
        return d

    def sb(self, shape, dt, es=None):
        self.nbuf += 1
        return Buf((es or self.es).enter_context(self.nc.sbuf_tensor("sb%d" % self.nbuf, shape, dt)))

    def _sync(self, eng, reads, writes):
        tags = []
        for r in reads:
            if r.w is not None:
                tags.append(r.w)
        for w in writes:
            if w.w is not None:
                tags.append(w.w)
            tags.extend(w.r.values())
        for (kind, obj, val) in tags:
            if kind == "e" and obj is eng and eng is self.pe:
                continue
            key = id(obj)
            if eng.seen.get(key, 0) >= val:
                continue
            eng.h.wait_ge(obj.sem, val)
            eng.seen[key] = val

    def I(self, eng, reads, writes, fn):
        reads = [b.r if isinstance(b, Buf) else b for b in reads]
        writes = [b.r if isinstance(b, Buf) else b for b in writes]
        self._sync(eng, reads, writes)
        ins = fn()
        eng.n += 1
        ins.then_inc(eng.sem, 1)
        tag = ("e", eng, eng.n)
        for r in reads:
            r.r[id(eng)] = tag
        for w in writes:
            w.w = tag
            w.r = {}
        return ins

    def dma(self, q, ds, reads, writes, fn):
        reads = [b.r if isinstance(b, Buf) else b for b in reads]
        writes = [b.r if isinstance(b, Buf) else b for b in writes]
        self._sync(q, reads, writes)
        ins = fn()
        ds.val += 16
        ins.then_inc(ds.sem, 16)
        tag = ("d", ds, ds.val)
        for r in reads:
            r.r[id(ds)] = tag
        for w in writes:
            w.w = tag
            w.r = {}
        return ins

    def barrier(self):
        for e in self.engs:
            for f in self.engs:
                if f.n == 0:
                    continue
                if e.seen.get(id(f), 0) < f.n:
                    e.h.wait_ge(f.sem, f.n)
                    e.seen[id(f)] = f.n
            for d in self.dss:
                if d.val and e.seen.get(id(d), 0) < d.val:
                    e.h.wait_ge(d.sem, d.val)
                    e.seen[id(d)] = d.val


def build(stop_after="G", taps=()):
    nc = bass.Bass("TRN2", target_bir_lowering=False)
    dti = lambda n, sh, dt: nc.dram_tensor(n, sh, dt, kind="ExternalInput").ap()
    dts = lambda n, sh, dt: nc.dram_tensor(n, sh, dt, kind="Internal").ap()
    x_d = dti("x", [S, D], F32)
    cT_d = dti("cT", [128, 8], F32)
    pos_d = dti("posT", [128, NT], I32)
    g1_d = dti("norm1_g", [1, D], F32)
    g2_d = dti("norm2_g", [1, D], F32)
    wada_d = dti("w_ada", [D, 6 * D], F32)
    bada_d = dti("b_ada", [1, 6 * D], F32)
    win_d = dti("w_in", [D, IN_COLS], F32)
    convw_d = dti("convw", [128, 12], F32)
    qg_d = dti("qg8", [1, 512], F32)
    kg_d = dti("kg8", [1, 512], F32)
    kig_d = dti("kig", [1, 64], F32)
    wout_d = dti("w_out", [D, D], F32)
    wr_d = dti("w_router", [D, NE], F32)
    rb_d = dti("router_bias", [1, NE], F32)
    w1_d = dti("w1", [NE, D, 256], F32)
    w3_d = dti("w3", [NE, D, 256], F32)
    w2_d = dti("w2", [NE, 256, D], F32)
    ws1_d = dti("ws1", [D, 256], F32)
    ws3_d = dti("ws3", [D, 256], F32)
    ws2_d = dti("ws2", [256, D], F32)
    out_d = nc.dram_tensor("out", [S, D], F32, kind="ExternalOutput").ap()
    tap_d = {n: nc.dram_tensor("tap_" + n, list(sh), dt, kind="ExternalOutput").ap() for (n, sh, dt) in taps}

    QT = dts("QT", [NT, 128, 512], BF16)
    QIT = dts("QIT", [NT, 128, 512], BF16)
    CONVT = dts("CONVT", [NT, 128, 512], BF16)
    ATTNT = dts("ATTNT", [NT, 128, 512], BF16)
    H2 = dts("H2", [S, D], BF16)
    BASE = dts("BASE", [S, D], F32)
    LIST = dts("LIST", [NSLOT, 1], I32)
    YS = dts("YS", [NSLOT, D], F32)
    WB13 = dts("WB13", [NE * 128, 4096], BF16)
    WB2 = dts("WB2", [NE * 128, 2048], BF16)
    ADA = dts("ADA", [128, 6 * D], F32)

    order = "ACDEFG"
    upto = lambda ph: order.index(ph) <= order.index(stop_after)

    with ExitStack() as es:
        kb = KB(nc, es)
        pe, act, dve, pool, sp = kb.pe, kb.act, kb.dve, kb.pool, kb.sp
        V, A, G, T = nc.vector, nc.scalar, nc.gpsimd, nc.tensor

        def tap(name, src_ap, reads, dst=None):
            if name not in tap_d:
                return
            d = kb.ds()
            kb.dma(sp, d, reads, [], lambda: nc.sync.dma_start(out=dst if dst is not None else tap_d[name], in_=src_ap))

        bk = [Buf(es.enter_context(nc.psum_tensor("bk%d" % i, [128, 512], F32))) for i in range(7)]
        PT = Buf(es.enter_context(nc.psum_tensor("PT", [128, 1024], BF16)))

        ident = kb.sb([128, 128], BF16)
        ident30 = kb.sb([128, 128], BF16)
        ones_bf = kb.sb([128, 128], BF16)
        ustrict = kb.sb([128, 128], BF16)
        ones_f = kb.sb([128, 128], F32)
        cbias = kb.sb([128, 128], BF16)
        eps = kb.sb([128, 1], F32)
        tok_all = kb.sb([128, NT], I32)
        piota = kb.sb([128, 1], F32)
        qg_bc = kb.sb([128, 512], F32)
        kg_bc = kb.sb([128, 512], F32)
        kig_bc = kb.sb([128, 64], F32)
        rb_bc = kb.sb([128, 64], F32)
        convw = kb.sb([128, 12], F32)
        wi_all = kb.sb([128, NT, 8], F32)
        g8_all = kb.sb([128, NT, 8], F32)
        s8_all = kb.sb([128, NT, 8], I32)
        cum_bc = kb.sb([128, NE], F32)
        widx = kb.sb([128, NB], I32)

        cds = kb.ds()

        def cload(q, buf, src, qh):
            kb.dma(q, kb.ds(), [], [buf], lambda: qh.dma_start(out=buf.t[:], in_=src))

        kb.I(pool, [], [ident], lambda: G.memset(ident.t[:], 1.0))
        kb.I(pool, [ident], [ident], lambda: G.affine_select(out=ident.t[:], in_=ident.t[:], pattern=[[1, 128]], compare_op=ALU.is_equal, fill=0.0, base=0, channel_multiplier=-1))
        kb.I(pool, [], [ident30], lambda: G.memset(ident30.t[:], 30000.0))
        kb.I(pool, [ident30], [ident30], lambda: G.affine_select(out=ident30.t[:], in_=ident30.t[:], pattern=[[1, 128]], compare_op=ALU.is_equal, fill=0.0, base=0, channel_multiplier=-1))
        kb.I(pool, [], [ones_bf], lambda: G.memset(ones_bf.t[:], 1.0))
        kb.I(pool, [], [ustrict], lambda: G.memset(ustrict.t[:], 1.0))
        kb.I(pool, [ustrict], [ustrict], lambda: G.affine_select(out=ustrict.t[:], in_=ustrict.t[:], pattern=[[1, 128]], compare_op=ALU.is_gt, fill=0.0, base=0, channel_multiplier=-1))
        kb.I(pool, [], [ones_f], lambda: G.memset(ones_f.t[:], 1.0))
        kb.I(pool, [], [cbias], lambda: G.memset(cbias.t[:], 0.0))
        kb.I(pool, [cbias], [cbias], lambda: G.affine_select(out=cbias.t[:], in_=cbias.t[:], pattern=[[-1, 128]], compare_op=ALU.is_ge, fill=-1e30, base=0, channel_multiplier=1))
        kb.I(pool, [], [eps], lambda: G.memset(eps.t[:], 1e-6))
        kb.I(pool, [], [tok_all], lambda: G.iota(tok_all.t[:], pattern=[[128, NT]], base=0, channel_multiplier=1))
        kb.I(pool, [], [piota], lambda: G.iota(piota.t[:], pattern=[[0, 1]], base=0, channel_multiplier=1, allow_small_or_imprecise_dtypes=True))
        kb.I(pool, [], [cum_bc], lambda: G.memset(cum_bc.t[:], 0.0))

        cload(sp, qg_bc, qg_d.partition_broadcast(128), nc.sync)
        cload(sp, kg_bc, kg_d.partition_broadcast(128), nc.sync)
        cload(sp, kig_bc, kig_d.partition_broadcast(128), nc.sync)
        cload(sp, rb_bc, rb_d.partition_broadcast(128), nc.sync)
        cload(sp, convw, convw_d[:, :], nc.sync)

        with ExitStack() as pa:
            ada = kb.sb([128, 6 * D], F32, pa)
            cT = kb.sb([128, 8], F32, pa)
            sg = kb.sb([128, 8], F32, pa)
            sc = kb.sb([128, 8], F32, pa)
            sc_bc = kb.sb([128, 8, 128], F32, pa)
            bada_bc = kb.sb([128, 6 * D], F32, pa)
            g_bc = kb.sb([128, 2 * D], F32, pa)
            wa = [kb.sb([128, 3 * D], F32, pa) for _ in range(2)]
            wads = [kb.ds(), kb.ds()]
            cload(sp, cT, cT_d[:, :], nc.sync)
            cload(sp, bada_bc, bada_d.partition_broadcast(128), nc.sync)
            gds_a = kb.ds()
            kb.dma(sp, gds_a, [], [g_bc], lambda: nc.sync.dma_start(out=g_bc.t[:, 0:D], in_=g1_d.partition_broadcast(128)))
            kb.dma(sp, gds_a, [g_bc], [g_bc], lambda: nc.sync.dma_start(out=g_bc.t[:, D:2 * D], in_=g2_d.partition_broadcast(128)))
            kb.I(act, [cT], [sg], lambda: A.activation(out=sg.t[:], in_=cT.t[:], func=AF.Sigmoid))
            kb.I(dve, [cT, sg], [sc], lambda: V.tensor_tensor(out=sc.t[:], in0=cT.t[:], in1=sg.t[:], op=ALU.mult))
            for kc in range(8):
                kb.I(dve, [sc, ones_f], [sc_bc], lambda: V.tensor_scalar(out=sc_bc.t[:, kc, :], in0=ones_f.t[:], scalar1=sc.t[:, kc:kc + 1], scalar2=None, op0=ALU.mult))
            it = 0
            for half in range(2):
                for kc in range(8):
                    wb_ = wa[it % 2]
                    kb.dma(sp, wads[it % 2], [], [wb_], lambda: nc.sync.dma_start(out=wb_.t[:], in_=wada_d[kc * 128:(kc + 1) * 128, half * 3 * D:(half + 1) * 3 * D]))
                    for g in range(6):
                        kb.I(pe, [sc_bc, wb_], [bk[g]], lambda: T.matmul(bk[g].t[:], lhsT=sc_bc.t[:, kc, :], rhs=wb_.t[:, g * 512:(g + 1) * 512], start=(kc == 0), stop=(kc == 7)))
                    it += 1
                for g in range(6):
                    c0 = half * 3 * D + g * 512
                    kb.I(dve, [bk[g], bada_bc], [ada], lambda: V.tensor_tensor(out=ada.t[:, c0:c0 + 512], in0=bk[g].t[:], in1=bada_bc.t[:, c0:c0 + 512], op=ALU.add))
            kb.I(dve, [ada, g_bc], [ada], lambda: V.scalar_tensor_tensor(out=ada.t[:, D:2 * D], in0=ada.t[:, D:2 * D], scalar=1.0, in1=g_bc.t[:, 0:D], op0=ALU.add, op1=ALU.mult))
            kb.I(dve, [ada, g_bc], [ada], lambda: V.scalar_tensor_tensor(out=ada.t[:, 4 * D:5 * D], in0=ada.t[:, 4 * D:5 * D], scalar=1.0, in1=g_bc.t[:, D:2 * D], op0=ALU.add, op1=ALU.mult))
            tap("ada", ada.t[0:1, :], [ada])
            kb.dma(sp, cds, [ada], [], lambda: nc.sync.dma_start(out=ADA[:, :], in_=ada.t[:]))
            kb.barrier()

        kside = ExitStack()
        kT = kb.sb([128, 4, S], BF16, kside)
        kiT2 = kb.sb([128, S], BF16, kside)
        Vp = kb.sb([128, NT, 4, 160], BF16, kside)
        kT_r = [Res() for _ in range(NT)]
        kiT_r = [Res() for _ in range(NT)]
        Vp_r = [Res() for _ in range(NT)]

        def rms_small(src_ap, n, scale, rs_out):
            pass

        if upto("C"):
          with ExitStack() as pc:
            Wbf = kb.sb([128, 8, IN_COLS], BF16, pc)
            ada = kb.sb([128, 2 * D], F32, pc)
            SHIFT1, A1 = slice(0, D), slice(D, 2 * D)
            cload(sp, ada, ADA[:, 0:2 * D], nc.sync)
            sincos = kb.sb([128, NT, 16], F32, pc)
            wds = kb.ds()
            for kc in range(8):
                kb.dma(pool, wds, [], [Wbf], lambda: G.dma_start(out=Wbf.t[:, kc, :], in_=win_d[kc * 128:(kc + 1) * 128, :], max_dma_last_dim=4096))
            prs = ExitStack()
            posi = kb.sb([128, NT], I32, prs)
            posf = kb.sb([128, NT], F32, prs)
            ang = kb.sb([128, NT, 16], F32, prs)
            angn = kb.sb([128, NT, 16], F32, prs)
            angi = kb.sb([128, NT, 16], I32, prs)
            angm = kb.sb([128, NT, 16], F32, prs)
            cload(sp, posi, pos_d[:, :], nc.sync)
            kb.I(dve, [posi], [posf], lambda: V.tensor_copy(out=posf.t[:], in_=posi.t[:]))
            for j in range(8):
                inv = float(np.float32(500000.0) ** np.float32(-j / 8.0))
                inv = float(np.float32(inv))
                kb.I(dve, [posf], [ang], lambda: V.tensor_scalar(out=ang.t[:, :, j], in0=posf.t[:], scalar1=inv, scalar2=None, op0=ALU.mult))
            kb.I(dve, [ang], [ang], lambda: V.tensor_scalar(out=ang.t[:, :, 8:16], in0=ang.t[:, :, 0:8], scalar1=math.pi / 2, scalar2=None, op0=ALU.add))
            kb.I(dve, [ang], [angn], lambda: V.tensor_scalar(out=angn.t[:], in0=ang.t[:], scalar1=1.0 / TWO_PI, scalar2=None, op0=ALU.mult))
            kb.I(dve, [angn], [angi], lambda: V.tensor_copy(out=angi.t[:], in_=angn.t[:]))
            kb.I(dve, [angi], [angn], lambda: V.tensor_copy(out=angn.t[:], in_=angi.t[:]))
            kb.I(dve, [angn, ang], [ang], lambda: V.scalar_tensor_tensor(out=ang.t[:], in0=angn.t[:], scalar=-TWO_PI, in1=ang.t[:], op0=ALU.mult, op1=ALU.add))
            kb.I(dve, [ang], [angm], lambda: V.tensor_scalar(out=angm.t[:], in0=ang.t[:], scalar1=-math.pi, scalar2=TWO_PI, op0=ALU.is_lt, op1=ALU.mult))
            kb.I(dve, [ang, angm], [ang], lambda: V.tensor_tensor(out=ang.t[:], in0=ang.t[:], in1=angm.t[:], op=ALU.add))
            kb.I(dve, [ang], [angm], lambda: V.tensor_scalar(out=angm.t[:], in0=ang.t[:], scalar1=math.pi, scalar2=-TWO_PI, op0=ALU.is_gt, op1=ALU.mult))
            kb.I(dve, [ang, angm], [ang], lambda: V.tensor_tensor(out=ang.t[:], in0=ang.t[:], in1=angm.t[:], op=ALU.add))
            kb.I(dve, [ang], [ang], lambda: V.tensor_scalar(out=ang.t[:], in0=ang.t[:], scalar1=3.1415925, scalar2=-3.1415925, op0=ALU.min, op1=ALU.max))
            kb.I(act, [ang], [sincos], lambda: A.activation(out=sincos.t[:], in_=ang.t[:], func=AF.Sin))
            tap("sincos", sincos.t[:], [sincos])
            kb.barrier()
            prs.close()
            kb.I(pool, [], Vp_r, lambda: G.memset(Vp.t[:], 0.0))
            kb.I(pool, [], Vp_r, lambda: G.memset(Vp.t[:, :, :, 64:65], 1.0))

            xbuf = [kb.sb([128, D], F32, pc) for _ in range(2)]
            xds = [kb.ds(), kb.ds()]
            ss = kb.sb([128, 1], F32, pc)
            sd = kb.sb([128, 1], F32, pc)
            rstd = kb.sb([128, 1], F32, pc)
            hb = kb.sb([128, D], BF16, pc)
            hT = [kb.sb([128, 8, 128], BF16, pc) for _ in range(2)]
            sq = kb.sb([128, 512], F32, pc)
            ssq = kb.sb([128, 8], F32, pc)
            sdq = kb.sb([128, 8], F32, pc)
            rq = kb.sb([128, 8], F32, pc)
            qn = kb.sb([128, 512], F32, pc)
            qb = kb.sb([128, 512], BF16, pc)
            kib = kb.sb([128, 128], BF16, pc)
            kin = kb.sb([128, 64], F32, pc)
            rt = [kb.sb([128, 64], F32, pc) for _ in range(4)]
            xcs = kb.sb([128, 512], F32, pc)
            uext = [kb.sb([128, 4, 130], F32, pc) for _ in range(2)]
            ytmp = kb.sb([128, 4, 128], F32, pc)
            stg = [kb.sb([128, 512], BF16, pc) for _ in range(6)]
            stg_ds = [kb.ds() for _ in range(6)]
            kb.I(pool, [], [uext[0]], lambda: G.memset(uext[0].t[:], 0.0))
            kb.I(pool, [], [uext[1]], lambda: G.memset(uext[1].t[:], 0.0))

            def rope(src3, dst3, tt, nh):
                co = sincos.t[:, tt, 8:16].rearrange("p (o j) -> p o j", o=1).to_broadcast([128, nh, 8])
                si = sincos.t[:, tt, 0:8].rearrange("p (o j) -> p o j", o=1).to_broadcast([128, nh, 8])
                x1 = src3[:, :, 0:8]
                x2 = src3[:, :, 8:16]
                v3 = lambda b: b.t[:, 0:nh * 8].rearrange("p (h j) -> p h j", j=8)
                return co, si, x1, x2, v3

            def do_rope(src_buf, src3, dst_buf, dst3, tt, nh):
                co, si, x1, x2, v3 = rope(src3, dst3, tt, nh)
                kb.I(dve, [src_buf, sincos], [rt[0]], lambda: V.tensor_tensor(out=v3(rt[0]), in0=x1, in1=co, op=ALU.mult))
                kb.I(dve, [src_buf, sincos], [rt[1]], lambda: V.tensor_tensor(out=v3(rt[1]), in0=x2, in1=si, op=ALU.mult))
                kb.I(dve, [src_buf, sincos], [rt[2]], lambda: V.tensor_tensor(out=v3(rt[2]), in0=x2, in1=co, op=ALU.mult))
                kb.I(dve, [src_buf, sincos], [rt[3]], lambda: V.tensor_tensor(out=v3(rt[3]), in0=x1, in1=si, op=ALU.mult))
                kb.I(dve, [rt[0], rt[1], dst_buf], [dst_buf], lambda: V.tensor_tensor(out=dst3[:, :, 0:8], in0=v3(rt[0]), in1=v3(rt[1]), op=ALU.subtract))
                kb.I(dve, [rt[2], rt[3], dst_buf], [dst_buf], lambda: V.tensor_tensor(out=dst3[:, :, 8:16], in0=v3(rt[2]), in1=v3(rt[3]), op=ALU.add))

            ssq17 = kb.sb([128, 17], F32, pc)
            sd17 = kb.sb([128, 17], F32, pc)
            rq17 = kb.sb([128, 17], F32, pc)

            def headstats(bank, o):
                kb.I(dve, [bank], [sq], lambda: V.tensor_tensor(out=sq.t[:], in0=bank.t[:], in1=bank.t[:], op=ALU.mult))
                kb.I(dve, [sq, ssq17], [ssq17], lambda: V.tensor_reduce(out=ssq17.t[:, o:o + 8], in_=sq.t[:].rearrange("p (h d) -> p h d", d=64), axis=AX.X, op=ALU.add))

            def headscale(bank, gbc, o):
                for hh in range(8):
                    cs = slice(hh * 64, (hh + 1) * 64)
                    kb.I(dve, [bank, rq17, gbc], [qn], lambda: V.scalar_tensor_tensor(out=qn.t[:, cs], in0=bank.t[:, cs], scalar=rq17.t[:, o + hh:o + hh + 1], in1=gbc.t[:, cs], op0=ALU.mult, op1=ALU.mult))

            hbb = [hb, kb.sb([128, D], BF16, pc)]
            qraw = kb.sb([128, 512], F32, pc)
            kraw = kb.sb([128, 512], F32, pc)
            qiraw = kb.sb([128, 512], F32, pc)
            kwraw = kb.sb([128, 72], F32, pc)
            bgraw = kb.sb([128, 512], F32, pc)
            kbq = kb.sb([128, 512], BF16, pc)
            qib = kb.sb([128, 512], BF16, pc)

            def stageA(tt):
                ts_ = slice(tt * 128, (tt + 1) * 128)
                xt = xbuf[tt % 2]
                hb_ = hbb[tt % 2]
                kb.dma(act, xds[tt % 2], [], [xt], lambda: nc.scalar.dma_start(out=xt.t[:], in_=x_d[ts_, :]))
                kb.I(act, [xt], [hb_, ss], lambda: A.activation(out=hb_.t[:], in_=xt.t[:], func=AF.Square, accum_out=ss.t[:, 0:1]))
                kb.I(act, [ss, eps], [sd], lambda: A.activation(out=sd.t[:], in_=ss.t[:], func=AF.Sqrt, scale=1.0 / D, bias=eps.t[:, 0:1]))
                kb.I(dve, [sd], [rstd], lambda: V.reciprocal(out=rstd.t[:], in_=sd.t[:]))
                kb.I(dve, [xt, rstd, ada], [xt], lambda: V.scalar_tensor_tensor(out=xt.t[:], in0=xt.t[:], scalar=rstd.t[:, 0:1], in1=ada.t[:, A1], op0=ALU.mult, op1=ALU.mult))
                kb.I(pool, [xt, ada], [hb_], lambda: G.tensor_tensor(out=hb_.t[:], in0=xt.t[:], in1=ada.t[:, SHIFT1], op=ALU.add))
                if tt == 0:
                    tap("h0", hb_.t[:], [hb_])

            col0 = [1536, 2048, 2560, 3072, 3584]
            wid = [512, 512, 512, 512, 72]
            bQ, bK, bV, bQI, bKW = bk[0], bk[1], bk[2], bk[3], bk[4]
            bXC, bBG, bCG = bk[5], bk[6], bk[2]

            def conv_mm(hTt, part, bb):
                for c in range(4):
                    cc = part * 512 + c * 128
                    for kc in range(8):
                        kb.I(pe, [hTt, Wbf], [bb], lambda: T.matmul(bb.t[:, c * 128:(c + 1) * 128], lhsT=Wbf.t[:, kc, cc:cc + 128], rhs=hTt.t[:, kc, :], start=(kc == 0), stop=(kc == 7)))

            def stageB(tt):
                hb_ = hbb[tt % 2]
                hTt = hT[tt % 2]
                for kc in range(8):
                    kb.I(pe, [hb_, ident], [PT], lambda: T.transpose(out=PT.t[:, kc * 128:(kc + 1) * 128], in_=hb_.t[:, kc * 128:(kc + 1) * 128], identity=ident.t[:]))
                kb.I(act, [PT], [hTt], lambda: A.copy(out=hTt.t[:].rearrange("p a b -> p (a b)"), in_=PT.t[:, :]))
                for gi in range(5):
                    for kc in range(8):
                        kb.I(pe, [hTt, Wbf], [bk[gi]], lambda: T.matmul(bk[gi].t[:, 0:wid[gi]], lhsT=hTt.t[:, kc, :], rhs=Wbf.t[:, kc, col0[gi]:col0[gi] + wid[gi]], start=(kc == 0), stop=(kc == 7)))
                conv_mm(hTt, 0, bXC)
                conv_mm(hTt, 1, bBG)
                bv3 = bV.t[:].rearrange("p (a b) -> p a b", b=128)
                kb.I(act, [bV], [Vp_r[tt]], lambda: A.copy(out=Vp.t[:, tt, :, 0:64], in_=bv3[:, :, 0:64]))
                kb.I(act, [bV], [Vp_r[tt]], lambda: A.copy(out=Vp.t[:, tt, :, 96:160], in_=bv3[:, :, 64:128]))
                kb.I(act, [bKW], [kwraw], lambda: A.copy(out=kwraw.t[:], in_=bKW.t[:, 0:72]))
                conv_mm(hTt, 2, bCG)

            def evac(tt):
                kb.I(act, [bQ], [qraw], lambda: A.copy(out=qraw.t[:], in_=bQ.t[:]))
                kb.I(act, [bK], [kraw], lambda: A.copy(out=kraw.t[:], in_=bK.t[:]))
                kb.I(act, [bQI], [qiraw], lambda: A.copy(out=qiraw.t[:], in_=bQI.t[:]))
                kb.I(act, [bXC], [xcs], lambda: A.copy(out=xcs.t[:], in_=bXC.t[:]))
                kb.I(act, [bBG], [bgraw], lambda: A.copy(out=bgraw.t[:], in_=bBG.t[:]))

            qn3 = qn.t[:].rearrange("p (h d) -> p h d", d=64)

            def post_dve(tt):
                ue, un = uext[tt % 2], uext[(tt + 1) % 2]
                kb.I(dve, [bCG, xcs], [ue], lambda: V.tensor_tensor(out=ue.t[:, :, 2:130], in0=bCG.t[:].rearrange("p (a b) -> p a b", b=128), in1=xcs.t[:].rearrange("p (a b) -> p a b", b=128), op=ALU.mult))
                kb.I(dve, [ue], [un], lambda: V.tensor_copy(out=un.t[:, :, 0:2], in_=ue.t[:, :, 128:130]))
                kb.I(pool, [kwraw], [wi_all], lambda: G.tensor_copy(out=wi_all.t[:, tt, :], in_=kwraw.t[:, 64:72]))
                headstats(qraw, 0)
                headstats(kraw, 8)
                kb.I(dve, [kwraw, ssq17], [sq, ssq17], lambda: V.scalar_tensor_tensor(out=sq.t[:, 0:64], in0=kwraw.t[:, 0:64], scalar=1.0, in1=kwraw.t[:, 0:64], op0=ALU.mult, op1=ALU.mult, accum_out=ssq17.t[:, 16:17]))
                kb.I(act, [ssq17, eps], [sd17], lambda: A.activation(out=sd17.t[:], in_=ssq17.t[:], func=AF.Sqrt, scale=1.0 / 64, bias=eps.t[:, 0:1]))
                kb.I(dve, [sd17], [rq17], lambda: V.reciprocal(out=rq17.t[:], in_=sd17.t[:]))
                headscale(qraw, qg_bc, 0)
                kb.I(pool, [qn], [qb], lambda: G.tensor_copy(out=qb.t[:], in_=qn.t[:]))
                do_rope(qn, qn3, qb, qb.t[:].rearrange("p (h d) -> p h d", d=64), tt, 8)
                if tt == 1:
                    tap("q1", qb.t[:], [qb])
                headscale(kraw, kg_bc, 8)
                kb.I(pool, [qn], [kbq], lambda: G.tensor_copy(out=kbq.t[:], in_=qn.t[:]))
                do_rope(qn, qn3, kbq, kbq.t[:].rearrange("p (h d) -> p h d", d=64), tt, 8)
                if tt == 1:
                    tap("k1", kbq.t[:], [kbq])
                kb.I(pool, [qiraw], [qib], lambda: G.tensor_copy(out=qib.t[:], in_=qiraw.t[:]))
                do_rope(qiraw, qiraw.t[:].rearrange("p (h d) -> p h d", d=64), qib, qib.t[:].rearrange("p (h d) -> p h d", d=64), tt, 8)
                kb.I(dve, [kwraw, rq17, kig_bc], [kin], lambda: V.scalar_tensor_tensor(out=kin.t[:], in0=kwraw.t[:, 0:64], scalar=rq17.t[:, 16:17], in1=kig_bc.t[:], op0=ALU.mult, op1=ALU.mult))
                kb.I(pool, [kin], [kib], lambda: G.tensor_copy(out=kib.t[:, 0:64], in_=kin.t[:]))
                do_rope(kin, kin.t[:].rearrange("p (h d) -> p h d", d=64), kib, kib.t[:, 0:64].rearrange("p (h d) -> p h d", d=64), tt, 1)
                kb.I(dve, [kib], [kib], lambda: V.tensor_copy(out=kib.t[:, 64:128], in_=kib.t[:, 0:64]))
                for c in range(4):
                    kb.I(dve, [ue, convw], [ytmp], lambda: V.tensor_scalar(out=ytmp.t[:, c, :], in0=ue.t[:, c, 2:130], scalar1=convw.t[:, c * 3 + 2:c * 3 + 3], scalar2=None, op0=ALU.mult))
                    kb.I(dve, [ue, convw, ytmp], [ytmp], lambda: V.scalar_tensor_tensor(out=ytmp.t[:, c, :], in0=ue.t[:, c, 1:129], scalar=convw.t[:, c * 3 + 1:c * 3 + 2], in1=ytmp.t[:, c, :], op0=ALU.mult, op1=ALU.add))
                    kb.I(dve, [ue, convw, ytmp], [ytmp], lambda: V.scalar_tensor_tensor(out=ytmp.t[:, c, :], in0=ue.t[:, c, 0:128], scalar=convw.t[:, c * 3:c * 3 + 1], in1=ytmp.t[:, c, :], op0=ALU.mult, op1=ALU.add))
                sc_ = stg[4 + tt % 2]
                kb.I(pool, [bgraw, ytmp], [sc_], lambda: G.tensor_tensor(out=sc_.t[:], in0=bgraw.t[:], in1=ytmp.t[:].rearrange("p a b -> p (a b)"), op=ALU.mult))
                kb.dma(sp, stg_ds[4 + tt % 2], [sc_], [], lambda: nc.sync.dma_start(out=CONVT[tt], in_=sc_.t[:]))

            def post_pe(tt):
                ts_ = slice(tt * 128, (tt + 1) * 128)
                sq_, sk_ = stg[tt % 2], stg[2 + tt % 2]
                for c in range(4):
                    kb.I(pe, [qb, ident], [PT], lambda: T.transpose(out=PT.t[:, c * 128:(c + 1) * 128], in_=qb.t[:, c * 128:(c + 1) * 128], identity=ident.t[:]))
                for c in range(4):
                    kb.I(pe, [kbq, ident], [PT], lambda: T.transpose(out=PT.t[:, 512 + c * 128:512 + (c + 1) * 128], in_=kbq.t[:, c * 128:(c + 1) * 128], identity=ident.t[:]))
                kb.I(act, [PT], [sq_], lambda: A.copy(out=sq_.t[:], in_=PT.t[:, 0:512]))
                kb.dma(sp, stg_ds[tt % 2], [sq_], [], lambda: nc.sync.dma_start(out=QT[tt], in_=sq_.t[:]))
                kb.I(act, [PT], [kT_r[tt]], lambda: A.copy(out=kT.t[:, :, ts_], in_=PT.t[:, 512:1024].rearrange("p (a b) -> p a b", b=128)))
                for c in range(4):
                    kb.I(pe, [qib, ident], [PT], lambda: T.transpose(out=PT.t[:, c * 128:(c + 1) * 128], in_=qib.t[:, c * 128:(c + 1) * 128], identity=ident.t[:]))
                kb.I(pe, [kib, ident], [PT], lambda: T.transpose(out=PT.t[:, 512:640], in_=kib.t[:], identity=ident.t[:]))
                kb.I(act, [PT], [sk_], lambda: A.copy(out=sk_.t[:], in_=PT.t[:, 0:512]))
                kb.dma(sp, stg_ds[2 + tt % 2], [sk_], [], lambda: nc.sync.dma_start(out=QIT[tt], in_=sk_.t[:]))
                kb.I(act, [PT], [kiT_r[tt]], lambda: A.copy(out=kiT2.t[:, ts_], in_=PT.t[:, 512:640]))

            stageA(0)
            for tt in range(NT):
                if tt + 1 < NT:
                    stageA(tt + 1)
                stageB(tt)
                evac(tt)
                if tt > 0:
                    post_pe(tt - 1)
                post_dve(tt)
            post_pe(NT - 1)
            tap("kT", kT.t[:, 0, 0:512], kT_r[0:4])
            tap("kiT", kiT2.t[:, 0:512], kiT_r[0:4])
            tap("wi", wi_all.t[:], [wi_all])
            kb.barrier()


        wb_state = {"e": 0}
        wbstage = []
        wbds = []

        def wb_step():
            e = wb_state["e"]
            if e >= NE:
                return
            wb_state["e"] = e + 1
            stg_ = wbstage[e % 2]
            dsl, dss_ = wbds[e % 2]
            kb.dma(pool, dsl, [], [stg_], lambda: G.dma_start(out=stg_.t[:, 0:2048].rearrange("p (c f) -> p c f", f=256), in_=w1_d[e].rearrange("(c p) f -> p c f", p=128)))
            kb.dma(pool, dsl, [stg_], [stg_], lambda: G.dma_start(out=stg_.t[:, 2048:4096].rearrange("p (c f) -> p c f", f=256), in_=w3_d[e].rearrange("(c p) f -> p c f", p=128)))
            kb.dma(pool, dsl, [stg_], [stg_], lambda: G.dma_start(out=stg_.t[:, 4096:6144].rearrange("p (c f) -> p c f", f=1024), in_=w2_d[e].rearrange("(c p) f -> p c f", p=128)))
            kb.dma(sp, dss_, [stg_], [], lambda: nc.sync.dma_start(out=WB13[e * 128:(e + 1) * 128, :], in_=stg_.t[:, 0:4096]))
            kb.dma(sp, dss_, [stg_], [], lambda: nc.sync.dma_start(out=WB2[e * 128:(e + 1) * 128, :], in_=stg_.t[:, 4096:6144]))

        if upto("D"):
          with ExitStack() as pd:
            wbstage.extend([kb.sb([128, 6144], BF16, pd) for _ in range(2)])
            wbds.extend([(kb.ds(), kb.ds()) for _ in range(2)])
            scoreb = [kb.sb([128, S], F32, pd) for _ in range(2)]
            mbb = [kb.sb([128, S], BF16, pd) for _ in range(2)]
            mTb = [kb.sb([128, S], BF16, pd) for _ in range(2)]
            dw = kb.sb([128, 8, 128], BF16, pd)
            qTb = [kb.sb([128, 512], BF16, pd) for _ in range(2)]
            qiTb = [kb.sb([128, 512], BF16, pd) for _ in range(2)]
            qds = [kb.ds() for _ in range(4)]
            Rb = [kb.sb([128, 512], BF16, pd) for _ in range(4)]
            pTb = [kb.sb([128, 512], BF16, pd) for _ in range(4)]
            lo = kb.sb([128, 1], F32, pd)
            hi = kb.sb([128, 1], F32, pd)
            w0 = kb.sb([128, 1], F32, pd)
            mid = kb.sb([128, 1], F32, pd)
            cnt = kb.sb([128, 1], F32, pd)
            tq = kb.sb([128, 1], F32, pd)
            rs = kb.sb([128, 512], F32, pd)
            bcs = kb.sb([128, 512], F32, pd)
            aob = [kb.sb([128, 512], BF16, pd) for _ in range(2)]
            aods = [kb.ds(), kb.ds()]
            kb.I(pool, [], [rs], lambda: G.memset(rs.t[:], 1.0))
            banksA = [bk[0], bk[1], bk[3], bk[4]]
            negbig = kb.sb([128, 1], F32, pd)
            kb.I(pool, [], [negbig], lambda: G.memset(negbig.t[:], -256.0))
            ident2k = kb.sb([128, 128], BF16, pd)
            kb.I(pool, [ident], [ident2k], lambda: G.tensor_scalar(out=ident2k.t[:], in0=ident.t[:], scalar1=2048.0, scalar2=1.0, op0=ALU.mult, op1=ALU.mult))
            psS = bk[2]
            STb = [bk[3], bk[4], bk[0], bk[1]]
            bankE, bankO = bk[5], bk[6]

            def load_qt(qt):
                a = qTb[qt % 2]
                kb.dma(act, qds[qt % 2], [], [a], lambda: nc.scalar.dma_start(out=a.t[:], in_=QT[qt]))

            def load_qi(qt):
                b_ = qiTb[qt % 2]
                kb.dma(act, qds[2 + qt % 2], [], [b_], lambda: nc.scalar.dma_start(out=b_.t[:], in_=QIT[qt]))

            def idx_phase(qt):
                qiT = qiTb[qt % 2]
                score = scoreb[qt % 2]
                Nk = 128 * (qt + 1)
                for h in range(8):
                    kb.I(pool, [ident, wi_all], [dw], lambda: G.tensor_scalar(out=dw.t[:, h, :], in0=ident.t[:], scalar1=wi_all.t[:, qt, h:h + 1], scalar2=1.0, op0=ALU.mult, op1=ALU.mult))
                wb_step()
                wb_step()
                nch = (Nk + 511) // 512
                for c_ in range(nch):
                    w = min(512, Nk - c_ * 512)
                    tiles = list(range(c_ * 4, c_ * 4 + w // 128))
                    krs = [kiT_r[t] for t in tiles]

                    def mmA(h):
                        hp, base = h // 2, 64 * (h % 2)
                        pa = banksA[h % 4]
                        kb.I(pe, [qiT] + krs, [pa], lambda: T.matmul(pa.t[:, 0:w], lhsT=qiT.t[base:base + 64, hp * 128:(hp + 1) * 128], rhs=kiT2.t[base:base + 64, c_ * 512:c_ * 512 + w], start=True, stop=True))
                    mmA(0)
                    mmA(1)
                    for p_ in range(4):
                        if p_ + 1 < 4:
                            mmA(2 * p_ + 2)
                            mmA(2 * p_ + 3)
                        for h in (2 * p_, 2 * p_ + 1):
                            pa = banksA[h % 4]
                            R = Rb[h % 4]
                            kb.I(act, [pa], [R], lambda: A.activation(out=R.t[:, 0:w], in_=pa.t[:, 0:w], func=AF.Relu))
                        for h in (2 * p_, 2 * p_ + 1):
                            R = Rb[h % 4]
                            kb.I(pe, [dw, R], [psS], lambda: T.matmul(psS.t[:, 0:w], lhsT=dw.t[:, h, :], rhs=R.t[:, 0:w], start=(h == 0), stop=(h == 7 and c_ != nch - 1)))
                    o0 = c_ * 512
                    if c_ == nch - 1:
                        kb.I(pe, [ident, cbias], [psS], lambda: T.matmul(psS.t[:, w - 128:w], lhsT=ident.t[:], rhs=cbias.t[:], start=False, stop=True))
                    kb.I(act, [psS], [score], lambda: A.copy(out=score.t[:, o0:o0 + w], in_=psS.t[:, 0:w]))

            def thr_phase(qt):
                Nk = 128 * (qt + 1)
                mbq = mbb[qt % 2]
                score = scoreb[qt % 2]
                if qt < 2:
                    kb.I(dve, [], [lo], lambda: V.memset(lo.t[:], -1e29))
                else:
                    n0 = qt * 128
                    kb.I(dve, [score], [hi], lambda: V.tensor_reduce(out=hi.t[:], in_=score.t[:, 0:Nk], axis=AX.X, op=ALU.max))
                    kb.I(dve, [score], [lo], lambda: V.tensor_reduce(out=lo.t[:], in_=score.t[:, 0:n0], axis=AX.X, op=ALU.min))
                    kb.I(dve, [lo], [lo], lambda: V.tensor_scalar(out=lo.t[:], in0=lo.t[:], scalar1=-1.0, scalar2=None, op0=ALU.add))
                    kb.I(dve, [hi, lo], [w0], lambda: V.tensor_tensor(out=w0.t[:], in0=hi.t[:], in1=lo.t[:], op=ALU.subtract))
                    for i in range(NIT):
                        f = 2.0 ** -(i + 1)
                        kb.I(dve, [w0, lo], [mid], lambda: V.scalar_tensor_tensor(out=mid.t[:], in0=w0.t[:], scalar=f, in1=lo.t[:], op0=ALU.mult, op1=ALU.add))
                        kb.I(dve, [score, mid], [mbq, cnt], lambda: V.tensor_scalar(out=mbq.t[:, 0:Nk], in0=score.t[:, 0:Nk], scalar1=mid.t[:, 0:1], scalar2=None, op0=ALU.is_gt, op1=ALU.add, accum_out=cnt.t[:, 0:1]))
                        kb.I(dve, [cnt], [tq], lambda: V.tensor_scalar(out=tq.t[:], in0=cnt.t[:], scalar1=NSEL - 0.5, scalar2=-1e30, op0=ALU.is_lt, op1=ALU.mult))
                        kb.I(dve, [tq, mid, lo], [lo], lambda: V.scalar_tensor_tensor(out=lo.t[:], in0=tq.t[:], scalar=mid.t[:, 0:1], in1=lo.t[:], op0=ALU.add, op1=ALU.max))
                kb.I(dve, [score, lo], [mbq], lambda: V.tensor_scalar(out=mbq.t[:, 0:Nk], in0=score.t[:, 0:Nk], scalar1=lo.t[:, 0:1], scalar2=None, op0=ALU.is_gt))

            def maskT_phase(qt):
                mbq, mT = mbb[qt % 2], mTb[qt % 2]
                nsb = qt + 1
                for g0 in range(0, nsb, 8):
                    n = min(8, nsb - g0)
                    for j in range(n):
                        sb_ = g0 + j
                        kb.I(pe, [mbq, ident], [PT], lambda: T.transpose(out=PT.t[:, j * 128:(j + 1) * 128], in_=mbq.t[:, sb_ * 128:(sb_ + 1) * 128], identity=ident.t[:]))
                    kb.I(act, [PT], [mT], lambda: A.copy(out=mT.t[:, g0 * 128:(g0 + n) * 128], in_=PT.t[:, 0:n * 128]))

            def attn_phase(qt):
                qTt = qTb[qt % 2]
                mT = mTb[qt % 2]
                mbq = mbb[qt % 2]
                nsb = qt + 1
                pairs = []
                for hp in range(4):
                    for g in range((nsb + 3) // 4):
                        pairs.append((hp, list(range(4 * g, min(4 * g + 4, nsb)))))

                def qkm(pi):
                    hp, sbs = pairs[pi]
                    pem = (pi % 2 == 0)
                    for j, sb_ in enumerate(sbs):
                        for par in range(2):
                            st = STb[(2 * pi + par) % 4]
                            base = 64 * par
                            kb.I(pe, [kT_r[sb_], qTt], [st], lambda: T.matmul(st.t[:, j * 128:(j + 1) * 128], lhsT=kT.t[base:base + 64, hp, sb_ * 128:(sb_ + 1) * 128], rhs=qTt.t[base:base + 64, hp * 128:(hp + 1) * 128], start=True, stop=not pem))
                        if pem:
                            for par in range(2):
                                st = STb[(2 * pi + par) % 4]
                                kb.I(pe, [mbq, ident2k], [st], lambda: T.matmul(st.t[:, j * 128:(j + 1) * 128], lhsT=mbq.t[:, sb_ * 128:(sb_ + 1) * 128], rhs=ident2k.t[:], start=False, stop=True))
                qkm(0)
                for pi, (hp, sbs) in enumerate(pairs):
                    if pi + 1 < len(pairs):
                        qkm(pi + 1)
                    pem = (pi % 2 == 0)
                    w = len(sbs) * 128
                    c0 = sbs[0] * 128
                    for par in range(2):
                        st = STb[(2 * pi + par) % 4]
                        pt = pTb[(2 * pi + par) % 4]
                        if pem:
                            kb.I(act, [st, negbig], [pt], lambda: A.activation(out=pt.t[:, 0:w], in_=st.t[:, 0:w], func=AF.Exp, scale=0.125, bias=negbig.t[:, 0:1]))
                        else:
                            kb.I(act, [st], [pt], lambda: A.activation(out=pt.t[:, 0:w], in_=st.t[:, 0:w], func=AF.Exp, scale=0.125))
                            kb.I(pool, [pt, mT], [pt], lambda: G.tensor_tensor(out=pt.t[:, 0:w], in0=pt.t[:, 0:w], in1=mT.t[:, c0:c0 + w], op=ALU.mult))
                    for par in range(2):
                        pt = pTb[(2 * pi + par) % 4]
                        for j, sb_ in enumerate(sbs):
                            if par == 0:
                                kb.I(pe, [Vp_r[sb_], pt], [bankE], lambda: T.matmul(bankE.t[0:65, hp * 128:(hp + 1) * 128], lhsT=Vp.t[:, sb_, hp, 0:65], rhs=pt.t[:, j * 128:(j + 1) * 128], start=(sb_ == 0), stop=(sb_ == nsb - 1)))
                            else:
                                kb.I(pe, [Vp_r[sb_], pt], [bankO], lambda: T.matmul(bankO.t[:, hp * 128:(hp + 1) * 128], lhsT=Vp.t[:, sb_, hp, 32:160], rhs=pt.t[:, j * 128:(j + 1) * 128], start=(sb_ == 0), stop=(sb_ == nsb - 1)))
                kb.I(dve, [bankE], [rs], lambda: V.reciprocal(out=rs.t[64:65, :], in_=bankE.t[64:65, :]))
                kb.I(dve, [bankO], [rs], lambda: V.reciprocal(out=rs.t[32:33, :], in_=bankO.t[32:33, :]))
                kb.I(pe, [rs, ones_f], [psS], lambda: T.matmul(psS.t[0:64, :], lhsT=ones_f.t[64:65, 0:64], rhs=rs.t[64:65, :], start=True, stop=True))
                kb.I(pe, [rs, ones_f], [psS], lambda: T.matmul(psS.t[64:128, :], lhsT=ones_f.t[32:33, 0:64], rhs=rs.t[32:33, :], start=True, stop=True))
                kb.I(act, [psS], [bcs], lambda: A.copy(out=bcs.t[:], in_=psS.t[:]))
                ao = aob[qt % 2]
                kb.I(dve, [bankE, bcs], [ao], lambda: V.tensor_tensor(out=ao.t[0:64, :], in0=bankE.t[0:64, :], in1=bcs.t[0:64, :], op=ALU.mult))
                kb.I(dve, [bankO, bcs, ao], [ao], lambda: V.tensor_tensor(out=ao.t[64:128, :], in0=bankO.t[64:128, :], in1=bcs.t[64:128, :], op=ALU.mult))
                kb.dma(sp, aods[qt % 2], [ao], [], lambda: nc.sync.dma_start(out=ATTNT[qt], in_=ao.t[:]))

            load_qi(0)
            load_qt(0)
            idx_phase(0)
            thr_phase(0)
            load_qi(1)
            idx_phase(1)
            for qt in range(NT):
                if qt + 1 < NT:
                    thr_phase(qt + 1)
                    load_qt(qt + 1)
                if qt + 2 < NT:
                    load_qi(qt + 2)
                    idx_phase(qt + 2)
                maskT_phase(qt)
                attn_phase(qt)
            kb.barrier()
            for tn in ("attn0", "attn5"):
                pass
            if "attnT" in tap_d:
                d_ = kb.ds()
                kb.dma(sp, d_, [], [], lambda: nc.sync.dma_start(out=tap_d["attnT"], in_=ATTNT))
            kb.barrier()


        kb.barrier()
        kside.close()
        if upto("E"):
          g_all = kb.sb([128, NT, NE], F32)
          pos_all = kb.sb([128, NT, NE], F32)
          with ExitStack() as pe_:
            adaE = kb.sb([128, 4 * D], F32, pe_)
            GATE1, SHIFT2, A2, GATE2 = [slice(i * D, (i + 1) * D) for i in range(4)]
            eds = kb.ds()
            kb.dma(sp, kb.ds(), [], [adaE], lambda: nc.sync.dma_start(out=adaE.t[:], in_=ADA[:, 2 * D:6 * D]))
            Wout = kb.sb([128, 8, D], BF16, pe_)
            Ws13 = kb.sb([128, 8, 512], BF16, pe_)
            Ws2 = kb.sb([128, 2, D], BF16, pe_)
            Wr = kb.sb([128, 8, NE], BF16, pe_)
            kb.dma(pool, kb.ds(), [], [Wout], lambda: G.dma_start(out=Wout.t[:], in_=wout_d.rearrange("(c p) f -> p c f", p=128)))
            kb.dma(pool, kb.ds(), [], [Wr], lambda: G.dma_start(out=Wr.t[:], in_=wr_d.rearrange("(c p) f -> p c f", p=128)))
            xbuf = [kb.sb([128, D], F32, pe_) for _ in range(2)]
            catb = [kb.sb([128, 8, 128], BF16, pe_) for _ in range(2)]
            lds = [kb.ds() for _ in range(2)]
            ldsx = [kb.ds() for _ in range(2)]
            tmp = kb.sb([128, D], F32, pe_)
            x1 = kb.sb([128, D], F32, pe_)
            junk = kb.sb([128, D], BF16, pe_)
            hf = kb.sb([128, D], F32, pe_)
            h2b = [kb.sb([128, D], BF16, pe_) for _ in range(2)]
            h2ds = [kb.ds(), kb.ds()]
            h2T = kb.sb([128, 8, 128], BF16, pe_)
            baseb = [kb.sb([128, D], F32, pe_) for _ in range(2)]
            bds = [kb.ds(), kb.ds()]
            ss = kb.sb([128, 1], F32, pe_)
            sd = kb.sb([128, 1], F32, pe_)
            rstd = kb.sb([128, 1], F32, pe_)
            scr = kb.sb([128, NE], F32, pe_)
            sel = kb.sb([128, NE], F32, pe_)
            m8 = kb.sb([128, 8], F32, pe_)
            msk = kb.sb([128, NE], F32, pe_)
            mskb = kb.sb([128, NE], BF16, pe_)
            gsel = kb.sb([128, NE], F32, pe_)
            gsum = kb.sb([128, 1], F32, pe_)
            rg = kb.sb([128, 1], F32, pe_)
            sgs = kb.sb([128, 256], F32, pe_)
            t1s = kb.sb([128, 256], F32, pe_)
            aT = kb.sb([128, 256], BF16, pe_)
            x1b = [x1, kb.sb([128, D], F32, pe_), kb.sb([128, D], F32, pe_)]
            h2Tb = [h2T, kb.sb([128, 8, 128], BF16, pe_)]
            tmp2 = kb.sb([128, D], F32, pe_)

            def stage1a(tt):
                x1_ = x1b[tt % 3]
                ts_ = slice(tt * 128, (tt + 1) * 128)
                xt, cat = xbuf[tt % 2], catb[tt % 2]
                kb.dma(act, ldsx[tt % 2], [], [xt], lambda: nc.scalar.dma_start(out=xt.t[:], in_=x_d[ts_, :]))
                kb.dma(act, lds[tt % 2], [], [cat], lambda: nc.scalar.dma_start(out=cat.t[:, 0:4, :], in_=CONVT[tt].rearrange("p (c t) -> p c t", t=128)))
                kb.dma(act, lds[tt % 2], [cat], [cat], lambda: nc.scalar.dma_start(out=cat.t[:, 4:8, :], in_=ATTNT[tt].rearrange("p (c t) -> p c t", t=128)))
                for hf_ in range(2):
                    for kc in range(8):
                        kb.I(pe, [cat, Wout], [bk[hf_]], lambda: T.matmul(bk[hf_].t[:], lhsT=cat.t[:, kc, :], rhs=Wout.t[:, kc, hf_ * 512:(hf_ + 1) * 512], start=(kc == 0), stop=(kc == 7)))
                for hf_ in range(2):
                    cs = slice(hf_ * 512, (hf_ + 1) * 512)
                    kb.I(dve, [bk[hf_], adaE], [tmp], lambda: V.tensor_tensor(out=tmp.t[:, cs], in0=bk[hf_].t[:], in1=adaE.t[:, hf_ * 512:(hf_ + 1) * 512], op=ALU.mult))
                kb.I(pool, [tmp, xt], [x1_], lambda: G.tensor_tensor(out=x1_.t[:], in0=tmp.t[:], in1=xt.t[:], op=ALU.add))
                if tt == 1:
                    tap("x1_1", x1_.t[:], [x1_])
                kb.I(act, [x1_], [junk, ss], lambda: A.activation(out=junk.t[:], in_=x1_.t[:], func=AF.Square, accum_out=ss.t[:, 0:1]))
                kb.I(act, [ss, eps], [sd], lambda: A.activation(out=sd.t[:], in_=ss.t[:], func=AF.Ln, scale=1.0 / D, bias=eps.t[:, 0:1]))
                kb.I(act, [sd], [rstd], lambda: A.activation(out=rstd.t[:], in_=sd.t[:], func=AF.Exp, scale=-0.5))
                kb.I(dve, [x1_, rstd, adaE], [hf], lambda: V.scalar_tensor_tensor(out=hf.t[:], in0=x1_.t[:], scalar=rstd.t[:, 0:1], in1=adaE.t[:, A2], op0=ALU.mult, op1=ALU.mult))
                hb2 = h2b[tt % 2]
                kb.I(pool, [hf, adaE], [hb2], lambda: G.tensor_tensor(out=hb2.t[:], in0=hf.t[:], in1=adaE.t[:, SHIFT2], op=ALU.add))
                kb.dma(sp, h2ds[tt % 2], [hb2], [], lambda: nc.sync.dma_start(out=H2[ts_, :], in_=hb2.t[:]))

            def stage1b(tt):
                hb2 = h2b[tt % 2]
                h2T_ = h2Tb[tt % 2]
                for kc in range(8):
                    kb.I(pe, [hb2, ident], [PT], lambda: T.transpose(out=PT.t[:, kc * 128:(kc + 1) * 128], in_=hb2.t[:, kc * 128:(kc + 1) * 128], identity=ident.t[:]))
                kb.I(act, [PT], [h2T_], lambda: A.copy(out=h2T_.t[:].rearrange("p a b -> p (a b)"), in_=PT.t[:, :]))

            def stage2(tt):
                ts_ = slice(tt * 128, (tt + 1) * 128)
                x1_ = x1b[tt % 3]
                h2T_ = h2Tb[tt % 2]
                for kc in range(8):
                    kb.I(pe, [h2T_, Wr], [bk[2]], lambda: T.matmul(bk[2].t[:, 0:NE], lhsT=h2T_.t[:, kc, :], rhs=Wr.t[:, kc, :], start=(kc == 0), stop=(kc == 7)))
                kb.I(act, [bk[2]], [scr], lambda: A.activation(out=scr.t[:], in_=bk[2].t[:, 0:NE], func=AF.Exp, scale=-1.0))
                kb.I(dve, [scr], [scr], lambda: V.tensor_scalar(out=scr.t[:], in0=scr.t[:], scalar1=1.0, scalar2=None, op0=ALU.add))
                kb.I(dve, [scr], [scr], lambda: V.reciprocal(out=scr.t[:], in_=scr.t[:]))
                kb.I(dve, [scr, rb_bc], [sel], lambda: V.tensor_tensor(out=sel.t[:], in0=scr.t[:], in1=rb_bc.t[:], op=ALU.add))
                kb.I(dve, [sel], [m8], lambda: V.max(out=m8.t[:], in_=sel.t[:]))
                kb.I(dve, [sel, m8], [msk], lambda: V.tensor_scalar(out=msk.t[:], in0=sel.t[:], scalar1=m8.t[:, 7:8], scalar2=None, op0=ALU.is_ge))
                kb.I(dve, [msk], [mskb], lambda: V.tensor_copy(out=mskb.t[:], in_=msk.t[:]))
                kb.I(pe, [ustrict, mskb], [bk[3]], lambda: T.matmul(bk[3].t[:, 0:NE], lhsT=ustrict.t[:], rhs=mskb.t[:], start=True, stop=True))
                kb.I(pe, [ones_bf, mskb], [bk[3]], lambda: T.matmul(bk[3].t[:, NE:2 * NE], lhsT=ones_bf.t[:], rhs=mskb.t[:], start=True, stop=True))
                kb.I(dve, [msk, scr], [gsel, gsum], lambda: V.scalar_tensor_tensor(out=gsel.t[:], in0=msk.t[:], scalar=1.0, in1=scr.t[:], op0=ALU.mult, op1=ALU.mult, accum_out=gsum.t[:, 0:1]))
                kb.I(dve, [gsum], [rg], lambda: V.reciprocal(out=rg.t[:], in_=gsum.t[:]))
                kb.I(dve, [gsel, rg], [g_all], lambda: V.tensor_scalar(out=g_all.t[:, tt, :], in0=gsel.t[:], scalar1=rg.t[:, 0:1], scalar2=2.5, op0=ALU.mult, op1=ALU.mult))
                kb.I(dve, [bk[3], cum_bc], [pos_all], lambda: V.tensor_tensor(out=pos_all.t[:, tt, :], in0=bk[3].t[:, 0:NE], in1=cum_bc.t[:], op=ALU.add))
                kb.I(dve, [bk[3], cum_bc], [cum_bc], lambda: V.tensor_tensor(out=cum_bc.t[:], in0=bk[3].t[:, NE:2 * NE], in1=cum_bc.t[:], op=ALU.add))
                kb.dma(sp, bds[tt % 2], [x1_], [], lambda: nc.sync.dma_start(out=BASE[ts_, :], in_=x1_.t[:]))

            stage1a(0)
            stage1a(1)
            stage1b(0)
            for tt in range(NT):
                if tt + 2 < NT:
                    stage1a(tt + 2)
                if tt + 1 < NT:
                    stage1b(tt + 1)
                stage2(tt)
            tap("g_all", g_all.t[:], [g_all])
            tap("pos_all", pos_all.t[:], [pos_all])
            kb.barrier()

        if upto("F"):
          with ExitStack() as p2:
            cnti = kb.sb([128, NE], I32, p2)
            padf = kb.sb([128, NE], F32, p2)
            cs_a = kb.sb([128, NE], F32, p2)
            cs_b = kb.sb([128, NE], F32, p2)
            pstart = kb.sb([128, NE], F32, p2)
            slotm = kb.sb([128, NE], F32, p2)
            mk_ = kb.sb([128, NE], F32, p2)
            s8f = kb.sb([128, 8], F32, p2)
            jk = kb.sb([128, NE], F32, p2)
            bef = kb.sb([128, NB], F32, p2)
            bstart = kb.sb([128, NB], F32, p2)
            skipf = kb.sb([128, NB], F32, p2)
            same2 = kb.sb([128, NB], F32, p2)
            zl = kb.sb([128, NSLOT // 128], I32, p2)
            li_a = kb.sb([128, 8], I32, p2)
            li_b = kb.sb([128, 8], I32, p2)
            lf_a = kb.sb([128, 8], F32, p2)
            lf_b = kb.sb([128, 8], F32, p2)
            li_all = kb.sb([128, NT, 8], I32, p2)
            lds_ = kb.ds()
            lds2_ = kb.ds()
            kb.I(pool, [], [zl], lambda: G.memset(zl.t[:], 1048576))
            list_r = Res()
            kb.dma(sp, lds_, [zl], [list_r], lambda: nc.sync.dma_start(out=LIST.rearrange("(p b) o -> p (b o)", p=128), in_=zl.t[:]))
            kb.I(dve, [cum_bc], [padf], lambda: V.tensor_scalar(out=padf.t[:], in0=cum_bc.t[:], scalar1=float(BS - 1), scalar2=None, op0=ALU.add))
            kb.I(dve, [padf], [cnti], lambda: V.tensor_copy(out=cnti.t[:], in_=padf.t[:]))
            kb.I(dve, [cnti], [cnti], lambda: V.tensor_scalar(out=cnti.t[:], in0=cnti.t[:], scalar1=8, scalar2=None, op0=ALU.arith_shift_right))
            kb.I(dve, [cnti], [cnti], lambda: V.tensor_scalar(out=cnti.t[:], in0=cnti.t[:], scalar1=8, scalar2=None, op0=ALU.logical_shift_left))
            kb.I(dve, [cnti], [padf], lambda: V.tensor_copy(out=padf.t[:], in_=cnti.t[:]))
            kb.I(dve, [padf], [cs_a], lambda: V.tensor_copy(out=cs_a.t[:], in_=padf.t[:]))
            cur, nxt = cs_a, cs_b
            sh = 1
            while sh < NE:
                kb.I(dve, [cur], [nxt], lambda: V.tensor_copy(out=nxt.t[:, 0:sh], in_=cur.t[:, 0:sh]))
                kb.I(dve, [cur, nxt], [nxt], lambda: V.tensor_tensor(out=nxt.t[:, sh:NE], in0=cur.t[:, sh:NE], in1=cur.t[:, 0:NE - sh], op=ALU.add))
                cur, nxt = nxt, cur
                sh *= 2
            pend = cur
            kb.I(dve, [pend, padf], [pstart], lambda: V.tensor_tensor(out=pstart.t[:], in0=pend.t[:], in1=padf.t[:], op=ALU.subtract))
            tap("pend", pend.t[0:1, :], [pend])
            Ws13 = kb.sb([128, 8, 512], BF16, p2)
            Ws2 = kb.sb([128, 2, D], BF16, p2)
            gate2s = kb.sb([128, D], F32, p2)
            sds = [kb.ds() for _ in range(3)]
            kb.dma(pool, sds[0], [], [Ws13], lambda: G.dma_start(out=Ws13.t[:, :, 0:256], in_=ws1_d.rearrange("(c p) f -> p c f", p=128)))
            kb.dma(pool, sds[0], [Ws13], [Ws13], lambda: G.dma_start(out=Ws13.t[:, :, 256:512], in_=ws3_d.rearrange("(c p) f -> p c f", p=128)))
            kb.dma(pool, sds[1], [], [Ws2], lambda: G.dma_start(out=Ws2.t[:], in_=ws2_d.rearrange("(c p) f -> p c f", p=128)))
            kb.dma(sp, sds[2], [], [gate2s], lambda: nc.sync.dma_start(out=gate2s.t[:], in_=ADA[:, 5 * D:6 * D]))
            shb = [kb.sb([128, D], BF16, p2) for _ in range(2)]
            sx1 = [kb.sb([128, D], F32, p2) for _ in range(2)]
            sld = [kb.ds(), kb.ds()]
            sld2 = [kb.ds(), kb.ds()]
            sh2T = kb.sb([128, 8, 128], BF16, p2)
            ssg = kb.sb([128, 256], F32, p2)
            st1 = kb.sb([128, 256], F32, p2)
            saT = kb.sb([128, 256], BF16, p2)
            stmp = kb.sb([128, D], F32, p2)
            sbase = [kb.sb([128, D], F32, p2) for _ in range(2)]
            sbd = [kb.ds(), kb.ds()]

            def sh_load(tt):
                ts_ = slice(tt * 128, (tt + 1) * 128)
                kb.dma(act, sld[tt % 2], [], [shb[tt % 2]], lambda: nc.scalar.dma_start(out=shb[tt % 2].t[:], in_=H2[ts_, :]))
                kb.dma(act, sld2[tt % 2], [], [sx1[tt % 2]], lambda: nc.scalar.dma_start(out=sx1[tt % 2].t[:], in_=BASE[ts_, :]))

            def sh_tile(tt):
                ts_ = slice(tt * 128, (tt + 1) * 128)
                hb_, x1_ = shb[tt % 2], sx1[tt % 2]
                for kc in range(8):
                    kb.I(pe, [hb_, ident], [PT], lambda: T.transpose(out=PT.t[:, kc * 128:(kc + 1) * 128], in_=hb_.t[:, kc * 128:(kc + 1) * 128], identity=ident.t[:]))
                kb.I(act, [PT], [sh2T], lambda: A.copy(out=sh2T.t[:].rearrange("p a b -> p (a b)"), in_=PT.t[:, :]))
                for j in range(4):
                    for kc in range(8):
                        kb.I(pe, [sh2T, Ws13], [bk[4]], lambda: T.matmul(bk[4].t[:, j * 128:(j + 1) * 128], lhsT=Ws13.t[:, kc, j * 128:(j + 1) * 128], rhs=sh2T.t[:, kc, :], start=(kc == 0), stop=(kc == 7)))
                kb.I(act, [bk[4]], [ssg], lambda: A.activation(out=ssg.t[:], in_=bk[4].t[:, 0:256], func=AF.Exp, scale=-1.0))
                kb.I(dve, [ssg], [ssg], lambda: V.tensor_scalar(out=ssg.t[:], in0=ssg.t[:], scalar1=1.0, scalar2=None, op0=ALU.add))
                kb.I(dve, [ssg], [ssg], lambda: V.reciprocal(out=ssg.t[:], in_=ssg.t[:]))
                kb.I(dve, [bk[4], ssg], [st1], lambda: V.tensor_tensor(out=st1.t[:], in0=bk[4].t[:, 0:256], in1=ssg.t[:], op=ALU.mult))
                kb.I(dve, [bk[4], st1], [saT], lambda: V.tensor_tensor(out=saT.t[:], in0=st1.t[:], in1=bk[4].t[:, 256:512], op=ALU.mult))
                for hf_ in range(2):
                    for fc in range(2):
                        kb.I(pe, [saT, Ws2], [bk[5 + hf_]], lambda: T.matmul(bk[5 + hf_].t[:], lhsT=saT.t[:, fc * 128:(fc + 1) * 128], rhs=Ws2.t[:, fc, hf_ * 512:(hf_ + 1) * 512], start=(fc == 0), stop=(fc == 1)))
                for hf_ in range(2):
                    cs = slice(hf_ * 512, (hf_ + 1) * 512)
                    kb.I(dve, [bk[5 + hf_], gate2s], [stmp], lambda: V.tensor_tensor(out=stmp.t[:, cs], in0=bk[5 + hf_].t[:], in1=gate2s.t[:, cs], op=ALU.mult))
                bs_ = sbase[tt % 2]
                kb.I(pool, [stmp, x1_], [bs_], lambda: G.tensor_tensor(out=bs_.t[:], in0=stmp.t[:], in1=x1_.t[:], op=ALU.add))
                kb.dma(sp, sbd[tt % 2], [bs_], [], lambda: nc.sync.dma_start(out=BASE[ts_, :], in_=bs_.t[:]))

            li_r = [Res() for _ in range(NT)]

            def e2_dve(tt):
                kb.I(dve, [g_all], [mk_], lambda: V.tensor_scalar(out=mk_.t[:], in0=g_all.t[:, tt, :], scalar1=0.0, scalar2=None, op0=ALU.is_gt))
                kb.I(dve, [pos_all, pstart], [slotm], lambda: V.scalar_tensor_tensor(out=slotm.t[:], in0=pos_all.t[:, tt, :], scalar=1.0, in1=pstart.t[:], op0=ALU.add, op1=ALU.add))
                kb.I(dve, [slotm, mk_], [slotm], lambda: V.tensor_tensor(out=slotm.t[:], in0=slotm.t[:], in1=mk_.t[:], op=ALU.mult))
                kb.I(dve, [slotm], [s8f], lambda: V.max(out=s8f.t[:], in_=slotm.t[:]))
                for j in range(8):
                    kb.I(dve, [slotm, s8f, g_all], [jk, g8_all], lambda: V.scalar_tensor_tensor(out=jk.t[:], in0=slotm.t[:], scalar=s8f.t[:, j:j + 1], in1=g_all.t[:, tt, :], op0=ALU.is_equal, op1=ALU.mult, accum_out=g8_all.t[:, tt, j:j + 1]))
                kb.I(dve, [s8f], [s8_all], lambda: V.tensor_scalar(out=s8_all.t[:, tt, :], in0=s8f.t[:], scalar1=-1.0, scalar2=0.0, op0=ALU.add, op1=ALU.max))
                kb.I(dve, [s8_all], [li_a], lambda: V.tensor_scalar(out=li_a.t[:], in0=s8_all.t[:, tt, :], scalar1=7, scalar2=None, op0=ALU.arith_shift_right))
                kb.I(dve, [s8_all], [li_b], lambda: V.tensor_scalar(out=li_b.t[:], in0=s8_all.t[:, tt, :], scalar1=127, scalar2=None, op0=ALU.bitwise_and))
                kb.I(dve, [li_a], [lf_a], lambda: V.tensor_copy(out=lf_a.t[:], in_=li_a.t[:]))
                kb.I(dve, [li_b], [lf_b], lambda: V.tensor_copy(out=lf_b.t[:], in_=li_b.t[:]))
                kb.I(dve, [lf_a, lf_b], [li_r[tt]], lambda: V.scalar_tensor_tensor(out=li_all.t[:, tt, :], in0=lf_b.t[:], scalar=float(NSLOT // 128), in1=lf_a.t[:], op0=ALU.mult, op1=ALU.add))

            def e2_scatter(tt):
                for j in range(8):
                    kb.dma(pool, lds2_, [li_r[tt], tok_all, list_r], [], lambda: G.indirect_dma_start(out=LIST, out_offset=bass.IndirectOffsetOnAxis(ap=li_all.t[:, tt, j:j + 1], axis=0), in_=tok_all.t[:, tt:tt + 1], in_offset=None))

            sh_load(0)
            e2_dve(0)
            for tt in range(NT):
                if tt + 1 < NT:
                    sh_load(tt + 1)
                    e2_dve(tt + 1)
                sh_tile(tt)
                e2_scatter(tt)
            for b in range(NB):
                kb.I(dve, [pend], [jk, bef], lambda: V.tensor_scalar(out=jk.t[:], in0=pend.t[:], scalar1=float(b * BS), scalar2=None, op0=ALU.is_le, op1=ALU.add, accum_out=bef.t[:, b:b + 1]))
            kb.I(pool, [], [bstart], lambda: G.iota(bstart.t[:], pattern=[[BS, NB]], base=0, channel_multiplier=0, allow_small_or_imprecise_dtypes=True))
            kb.I(dve, [bstart, pend], [skipf], lambda: V.tensor_scalar(out=skipf.t[:], in0=bstart.t[:], scalar1=pend.t[:, NE - 1:NE], scalar2=None, op0=ALU.is_ge))
            kb.I(dve, [bef], [same2], lambda: V.memset(same2.t[:], 0.0))
            kb.I(dve, [bef, same2], [same2], lambda: V.tensor_tensor(out=same2.t[:, 2:NB], in0=bef.t[:, 2:NB], in1=bef.t[:, 0:NB - 2], op=ALU.is_equal))
            kb.I(dve, [skipf, same2], [skipf], lambda: V.tensor_tensor(out=skipf.t[:], in0=skipf.t[:], in1=same2.t[:], op=ALU.max))
            kb.I(dve, [bef], [bef], lambda: V.tensor_scalar(out=bef.t[:], in0=bef.t[:], scalar1=float(NE - 1), scalar2=128.0, op0=ALU.min, op1=ALU.mult))
            kb.I(dve, [bef, skipf], [bef], lambda: V.scalar_tensor_tensor(out=bef.t[:], in0=skipf.t[:], scalar=1048576.0, in1=bef.t[:], op0=ALU.mult, op1=ALU.add))
            kb.I(dve, [bef, piota], [widx], lambda: V.tensor_scalar(out=widx.t[:], in0=bef.t[:], scalar1=piota.t[:, 0:1], scalar2=None, op0=ALU.add))
            tap("g8", g8_all.t[:], [g8_all])
            tap("s8", s8_all.t[:], [s8_all])
            tap("widx", widx.t[:], [widx])
            while wb_state["e"] < NE:
                if not wbstage:
                    wbstage.extend([kb.sb([128, 6144], BF16, p2) for _ in range(2)])
                    wbds.extend([(kb.ds(), kb.ds()) for _ in range(2)])
                wb_step()
            kb.barrier()

        if upto("F"):
          with ExitStack() as pf:
            reg_w = G.to_reg(NE * 128 - 1)
            reg_t = G.to_reg(S - 1)
            w13b = [kb.sb([128, 4096], BF16, pf) for _ in range(2)]
            w2b = [kb.sb([128, 2048], BF16, pf) for _ in range(2)]
            wgds = [kb.ds() for _ in range(4)]
            tl_all = kb.sb([128, NSLOT // 128], I32, pf)
            tlds = kb.ds()
            kb.dma(sp, tlds, [], [tl_all], lambda: nc.sync.dma_start(out=tl_all.t[:], in_=LIST.rearrange("(p b) o -> p (b o)", p=128)))
            xg = [kb.sb([128, D], BF16, pf) for _ in range(4)]
            xgds = [kb.ds() for _ in range(4)]
            xgT = [kb.sb([128, 8, 256], BF16, pf) for _ in range(2)]
            for xb_ in xg:
                kb.I(pool, [], [xb_], lambda: G.memset(xb_.t[:], 0.0))
            sg = kb.sb([128, 512], F32, pf)
            t1 = kb.sb([128, 512], F32, pf)
            aT = [kb.sb([128, 512], BF16, pf) for _ in range(2)]
            ysb = [kb.sb([128, D], F32, pf) for _ in range(4)]
            yds = [kb.ds() for _ in range(4)]
            def gath_w13(b):
                wb_ = w13b[b % 2]
                kb.dma(pool, wgds[b % 2], [widx], [wb_], lambda: G.indirect_dma_start(out=wb_.t[:], out_offset=None, in_=WB13, in_offset=bass.IndirectOffsetOnAxis(ap=widx.t[:, b:b + 1], axis=0), bounds_check=reg_w, oob_is_err=False))

            def gath_w2(b):
                wb_ = w2b[b % 2]
                kb.dma(pool, wgds[2 + b % 2], [widx], [wb_], lambda: G.indirect_dma_start(out=wb_.t[:], out_offset=None, in_=WB2, in_offset=bass.IndirectOffsetOnAxis(ap=widx.t[:, b:b + 1], axis=0), bounds_check=reg_w, oob_is_err=False))

            def gath_x(b):
                for sk in range(2):
                    i4 = (2 * b + sk) % 4
                    kb.dma(pool, xgds[i4], [tl_all], [xg[i4]], lambda: G.indirect_dma_start(out=xg[i4].t[:], out_offset=None, in_=H2, in_offset=bass.IndirectOffsetOnAxis(ap=tl_all.t[:, 2 * b + sk:2 * b + sk + 1], axis=0), bounds_check=reg_t, oob_is_err=False))

            PT2 = bk[6].t[:].bitcast(BF16)

            def tr(b):
                xT = xgT[b % 2]
                for sk in range(2):
                    i4 = (2 * b + sk) % 4
                    if sk == 0:
                        for kc in range(8):
                            kb.I(pe, [xg[i4], ident], [PT], lambda: T.transpose(out=PT.t[:, kc * 128:(kc + 1) * 128], in_=xg[i4].t[:, kc * 128:(kc + 1) * 128], identity=ident.t[:]))
                        kb.I(act, [PT], [xT], lambda: A.copy(out=xT.t[:, :, 0:128], in_=PT.t[:, :].rearrange("p (a b) -> p a b", b=128)))
                    else:
                        for kc in range(8):
                            kb.I(pe, [xg[i4], ident], [bk[6]], lambda: T.transpose(out=PT2[:, kc * 128:(kc + 1) * 128], in_=xg[i4].t[:, kc * 128:(kc + 1) * 128], identity=ident.t[:]))
                        kb.I(dve, [bk[6], xT], [xT], lambda: V.tensor_copy(out=xT.t[:, :, 128:256], in_=PT2.rearrange("p (a b) -> p a b", b=128)))

            def hmm(b):
                wb_, xT = w13b[b % 2], xgT[b % 2]
                for m_ in range(2):
                    for fc in range(2):
                        for kc in range(8):
                            kb.I(pe, [wb_, xT], [bk[m_]], lambda: T.matmul(bk[m_].t[:, fc * 256:(fc + 1) * 256], lhsT=wb_.t[:, m_ * 2048 + kc * 256 + fc * 128:m_ * 2048 + kc * 256 + (fc + 1) * 128], rhs=xT.t[:, kc, :], start=(kc == 0), stop=(kc == 7)))

            def silu(b):
                a_ = aT[b % 2]
                kb.I(act, [bk[0]], [sg], lambda: A.activation(out=sg.t[:], in_=bk[0].t[:], func=AF.Sigmoid))
                kb.I(dve, [bk[0], sg], [t1], lambda: V.tensor_tensor(out=t1.t[:], in0=bk[0].t[:], in1=sg.t[:], op=ALU.mult))
                kb.I(dve, [bk[1], t1], [a_], lambda: V.tensor_tensor(out=a_.t[:], in0=t1.t[:], in1=bk[1].t[:], op=ALU.mult))

            def w2(b):
                wb_, a_ = w2b[b % 2], aT[b % 2]
                for sk in range(2):
                    yb = ysb[(2 * b + sk) % 4]
                    for hf_ in range(2):
                        ob = bk[2 + 2 * sk + hf_]
                        for fc in range(2):
                            kb.I(pe, [a_, wb_], [ob], lambda: T.matmul(ob.t[:], lhsT=a_.t[:, fc * 256 + sk * 128:fc * 256 + (sk + 1) * 128], rhs=wb_.t[:, fc * 1024 + hf_ * 512:fc * 1024 + (hf_ + 1) * 512], start=(fc == 0), stop=(fc == 1)))
                    kb.I(act, [bk[2 + 2 * sk]], [yb], lambda: A.copy(out=yb.t[:, 0:512], in_=bk[2 + 2 * sk].t[:]))
                    kb.I(dve, [bk[3 + 2 * sk], yb], [yb], lambda: V.tensor_copy(out=yb.t[:, 512:1024], in_=bk[3 + 2 * sk].t[:]))
                    r0 = b * BS + sk * 128
                    kb.dma(sp, yds[(2 * b + sk) % 4], [yb], [], lambda: nc.sync.dma_start(out=YS[r0:r0 + 128, :], in_=yb.t[:]))

            gath_w13(0)
            gath_x(0)
            gath_w13(1)
            gath_w2(0)
            gath_x(1)
            gath_w2(1)
            tr(0)
            hmm(0)
            gath_w13(2)
            for b in range(NB):
                silu(b)
                if b + 2 < NB:
                    gath_x(b + 2)
                if b + 1 < NB:
                    tr(b + 1)
                w2(b)
                if b + 2 < NB:
                    gath_w2(b + 2)
                if b + 1 < NB:
                    hmm(b + 1)
                if b + 3 < NB:
                    gath_w13(b + 3)
            kb.barrier()

        if upto("G"):
          with ExitStack() as pg:
            gate2 = kb.sb([128, D], F32, pg)
            gds_ = kb.ds()
            kb.dma(sp, gds_, [], [gate2], lambda: nc.sync.dma_start(out=gate2.t[:], in_=ADA[:, 5 * D:6 * D]))
            baseb = [kb.sb([128, D], F32, pg) for _ in range(3)]
            bds = [kb.ds(), kb.ds(), kb.ds()]
            yg = [[kb.sb([128, D], F32, pg) for _ in range(8)] for _ in range(3)]
            ygds = [kb.ds() for _ in range(3)]
            acc = kb.sb([128, D], F32, pg)
            ob_ = [kb.sb([128, D], F32, pg) for _ in range(2)]
            ods = [kb.ds(), kb.ds()]
            def g_load(tt):
                ts_ = slice(tt * 128, (tt + 1) * 128)
                p_, q_ = tt % 2, tt % 3
                kb.dma(sp, bds[q_], [], [baseb[q_]], lambda: nc.sync.dma_start(out=baseb[q_].t[:], in_=BASE[ts_, :]))
                for j in range(8):
                    kb.dma(pool, ygds[q_], [s8_all], [yg[q_][j]], lambda: G.indirect_dma_start(out=yg[q_][j].t[:], out_offset=None, in_=YS, in_offset=bass.IndirectOffsetOnAxis(ap=s8_all.t[:, tt, j:j + 1], axis=0)))
                for j in range(8):
                    yg[q_][j].r.w = yg[q_][7].r.w

            g_load(0)
            g_load(1)
            for tt in range(NT):
                ts_ = slice(tt * 128, (tt + 1) * 128)
                p_, q_ = tt % 2, tt % 3
                if tt + 2 < NT:
                    g_load(tt + 2)
                pb = (bk[0], bk[1]) if tt % 2 == 0 else (bk[2], bk[3])
                for hf_ in range(2):
                    cs = slice(hf_ * 512, (hf_ + 1) * 512)
                    kb.I(dve, [yg[q_][0], g8_all], [pb[hf_]], lambda: V.tensor_scalar(out=pb[hf_].t[:], in0=yg[q_][0].t[:, cs], scalar1=g8_all.t[:, tt, 0:1], scalar2=None, op0=ALU.mult))
                for j in range(1, 8):
                    for hf_ in range(2):
                        cs = slice(hf_ * 512, (hf_ + 1) * 512)
                        kb.I(dve, [yg[q_][j], g8_all, pb[hf_]], [pb[hf_]], lambda: V.scalar_tensor_tensor(out=pb[hf_].t[:], in0=yg[q_][j].t[:, cs], scalar=g8_all.t[:, tt, j:j + 1], in1=pb[hf_].t[:], op0=ALU.mult, op1=ALU.add))
                for hf_ in range(2):
                    cs = slice(hf_ * 512, (hf_ + 1) * 512)
                    kb.I(dve, [pb[hf_], gate2], [acc], lambda: V.tensor_tensor(out=acc.t[:, cs], in0=pb[hf_].t[:], in1=gate2.t[:, cs], op=ALU.mult))
                o_ = ob_[p_]
                kb.I(pool, [acc, baseb[q_]], [o_], lambda: G.tensor_tensor(out=o_.t[:], in0=acc.t[:], in1=baseb[q_].t[:], op=ALU.add))
                kb.dma(sp, ods[p_], [o_], [], lambda: nc.sync.dma_start(out=out_d[ts_, :], in_=o_.t[:]))
            kb.barrier()

        kb.barrier()
    return nc


def _prep_inputs(inputs):
    f = np.float32
    common = {
        "norm1_g": np.ascontiguousarray(inputs["norm1_g"], f).reshape(1, D),
        "norm2_g": np.ascontiguousarray(inputs["norm2_g"], f).reshape(1, D),
        "w_ada": np.ascontiguousarray(inputs["w_ada"][0], f),
        "b_ada": np.ascontiguousarray(inputs["b_ada"], f).reshape(1, 6 * D),
        "w_in": np.ascontiguousarray(inputs["w_in"][0], f),
        "convw": np.ascontiguousarray(inputs["conv_w"][0].reshape(3, 4, 128).transpose(2, 1, 0).reshape(128, 12), f),
        "qg8": np.ascontiguousarray(np.tile(inputs["q_norm_g"][0], 8).reshape(1, 512), f),
        "kg8": np.ascontiguousarray(np.tile(inputs["k_norm_g"][0], 8).reshape(1, 512), f),
        "kig": np.ascontiguousarray(inputs["kidx_norm_g"], f).reshape(1, 64),
        "w_out": np.ascontiguousarray(inputs["w_out"][0], f),
        "w_router": np.ascontiguousarray(inputs["w_router"][0], f),
        "router_bias": np.ascontiguousarray(inputs["router_bias"], f).reshape(1, NE),
        "w1": np.ascontiguousarray(inputs["w1"][0], f),
        "w3": np.ascontiguousarray(inputs["w3"][0], f),
        "w2": np.ascontiguousarray(inputs["w2"][0], f),
        "ws1": np.ascontiguousarray(inputs["ws1"][0], f),
        "ws3": np.ascontiguousarray(inputs["ws3"][0], f),
        "ws2": np.ascontiguousarray(inputs["ws2"][0], f),
    }
    maps = []
    for b in range(8):
        m = dict(common)
        m["x"] = np.ascontiguousarray(inputs["x"][b], f)
        m["cT"] = np.ascontiguousarray(inputs["c"][b].reshape(8, 128).T, f)
        m["posT"] = np.ascontiguousarray(inputs["positions"][b].reshape(NT, 128).T.astype(np.int32))
        maps.append(m)
    return maps


def kernel(**inputs):
    maps = _prep_inputs(inputs)
    nc = build()
    res = run_bass_kernel_spmd(nc, maps, core_ids=list(range(8)))
    return np.stack([np.asarray(r["out"], dtype=np.float32) for r in res.results], axis=0)
```
